# Optimizing a Trainium2 kernel written in Bass

```python
import math
import jax, jax.numpy as jnp
from jax import lax
import numpy as np

D_MODEL = 1024
BATCH = 16
SEQ = 2048
DEPTH = 2

N_EVEN = (DEPTH + 1) // 2
N_ODD = DEPTH // 2
EPS = 1e-6

CONF_WIDTH = D_MODEL // 2
CONF_KERNEL = 31
LRU_WIDTH = D_MODEL // 2
LRU_HEADS = 8
LRU_HEAD_DIM = LRU_WIDTH // LRU_HEADS
LRU_CONV = 4
LRU_C = 8.0
AB_IN = 2 * CONF_WIDTH + 2 * LRU_WIDTH
AB_OUT = CONF_WIDTH + LRU_WIDTH
ML_HEADS = 8
ML_V_DIM = D_MODEL
ML_HEAD_V = ML_V_DIM // ML_HEADS
ML_HEAD_QK = ML_HEAD_V // 2
ML_QK_DIM = ML_HEADS * ML_HEAD_QK
ML_CHUNK = 64
ML_IN = 2 * ML_QK_DIM + 2 * ML_V_DIM + 2 * ML_HEADS
D_FF = int(math.ceil(8 * D_MODEL / 3 / 256)) * 256

kernel_name = 'hybrid_conformer_rglru_mlstm_block'


def _rmsnorm(x, g):
    xf = x.astype(jnp.float32)
    y = xf * lax.rsqrt(jnp.mean(xf * xf, axis=-1, keepdims=True) + EPS)
    return (y * g.astype(jnp.float32)).astype(x.dtype)


def _layernorm(x, g, b):
    xf = x.astype(jnp.float32)
    mu = jnp.mean(xf, axis=-1, keepdims=True)
    var = jnp.mean(jnp.square(xf - mu), axis=-1, keepdims=True)
    y = (xf - mu) * lax.rsqrt(var + EPS)
    return (y * g.astype(jnp.float32) + b.astype(jnp.float32)).astype(x.dtype)


def _causal_dwconv(x, w, b):
    k = w.shape[0]
    c = x.shape[-1]
    y = lax.conv_general_dilated(
        x, w[:, None, :].astype(x.dtype), window_strides=(1,), padding=[(k - 1, 0)],
        dimension_numbers=('NWC', 'WIO', 'NWC'), feature_group_count=c)
    return y + b


def _conformer_conv(u, conv_w, conv_b, ln_g, ln_b):
    val, gate = jnp.split(u, 2, axis=-1)
    y = val * jax.nn.sigmoid(gate)
    y = _causal_dwconv(y, conv_w, conv_b)
    return jax.nn.silu(_layernorm(y, ln_g, ln_b))


def _lru_combine(left, right):
    a_l, b_l = left
    a_r, b_r = right
    return a_l * a_r, a_r * b_l + b_r


def _rglru_block(u, conv_w, conv_b, w_a, b_a, w_x, b_x, lam):
    bsz, seq, _ = u.shape
    gate_in, rec_in = jnp.split(u, 2, axis=-1)
    xr = _causal_dwconv(rec_in, conv_w, conv_b)
    xh = xr.reshape(bsz, seq, LRU_HEADS, LRU_HEAD_DIM)
    r = jax.nn.sigmoid(jnp.einsum('bshd,hde->bshe', xh, w_a) + b_a)
    i = jax.nn.sigmoid(jnp.einsum('bshd,hde->bshe', xh, w_x) + b_x)
    log_base = -jax.nn.softplus(-lam.astype(jnp.float32)).reshape(LRU_HEADS, LRU_HEAD_DIM)
    log_a = LRU_C * log_base * r.astype(jnp.float32)
    a = jnp.exp(log_a)
    gated_x = jnp.sqrt(-jnp.expm1(2.0 * log_a)) * (i * xh).astype(jnp.float32)
    _, h = lax.associative_scan(_lru_combine, (a, gated_x), axis=1)
    h = h.reshape(bsz, seq, LRU_WIDTH).astype(u.dtype)
    return h * jax.nn.gelu(gate_in)


def _ab_mixer(xn, w_in, b_in, conf_conv_w, conf_conv_b, conf_ln_g, conf_ln_b,
              lru_conv_w, lru_conv_b, lru_w_a, lru_b_a, lru_w_x, lru_b_x, lru_lambda, w_out):
    u = jnp.einsum('bsd,de->bse', xn, w_in) + b_in
    u_conf, u_lru = jnp.split(u, [2 * CONF_WIDTH], axis=-1)
    y_a = _conformer_conv(u_conf, conf_conv_w, conf_conv_b, conf_ln_g, conf_ln_b)
    y_b = _rglru_block(u_lru, lru_conv_w, lru_conv_b, lru_w_a, lru_b_a, lru_w_x, lru_b_x, lru_lambda)
    y = jnp.concatenate([y_a, y_b], axis=-1)
    return jnp.einsum('bse,ed->bsd', y, w_out)


def _mlstm_chunk_step(carry, inp):
    c_state, n_state, m_state = carry
    qc, kc, vc, ic, lfc = inp
    L = qc.shape[2]
    causal = jnp.tril(jnp.ones((L, L), dtype=bool))
    b = jnp.cumsum(lfc, axis=-1)
    log_d = b[..., :, None] - b[..., None, :] + ic[..., None, :]
    log_d = jnp.where(causal, log_d, -jnp.inf)
    log_inter = b + m_state[..., None]
    m_row = jnp.maximum(log_inter, jnp.max(log_d, axis=-1))
    d_w = jnp.exp(log_d - m_row[..., None])
    inter_w = jnp.exp(log_inter - m_row)
    scores = jnp.einsum('bhjd,bhsd->bhjs', qc, kc) * d_w
    num = jnp.einsum('bhjs,bhsv->bhjv', scores, vc) + inter_w[..., None] * jnp.einsum('bhjd,bhdv->bhjv', qc, c_state)
    den = jnp.sum(scores, axis=-1) + inter_w * jnp.einsum('bhjd,bhd->bhj', qc, n_state)
    h = num / jnp.maximum(jnp.abs(den), jnp.exp(-m_row))[..., None]
    b_last = b[..., -1]
    log_w = b_last[..., None] - b + ic
    m_new = jnp.maximum(b_last + m_state, jnp.max(log_w, axis=-1))
    w_s = jnp.exp(log_w - m_new[..., None])
    decay = jnp.exp(b_last + m_state - m_new)
    c_new = decay[..., None, None] * c_state + jnp.einsum('bhsd,bhsv->bhdv', w_s[..., None] * kc, vc)
    n_new = decay[..., None] * n_state + jnp.einsum('bhs,bhsd->bhd', w_s, kc)
    return (c_new, n_new, m_new), h


def _mlstm_mixer(xn, w_in, b_in, head_g, w_out):
    bsz, seq, _ = xn.shape
    nc = seq // ML_CHUNK
    u = jnp.einsum('bsd,de->bse', xn, w_in) + b_in
    q, k, v, o, ig, fg = jnp.split(
        u, [ML_QK_DIM, 2 * ML_QK_DIM, 2 * ML_QK_DIM + ML_V_DIM,
            2 * ML_QK_DIM + 2 * ML_V_DIM, 2 * ML_QK_DIM + 2 * ML_V_DIM + ML_HEADS], axis=-1)
    f32 = jnp.float32

    def to_chunks(t, d):
        return t.astype(f32).reshape(bsz, nc, ML_CHUNK, ML_HEADS, d).transpose(1, 0, 3, 2, 4)

    def gate_chunks(t):
        return t.astype(f32).reshape(bsz, nc, ML_CHUNK, ML_HEADS).transpose(1, 0, 3, 2)

    qc = to_chunks(q, ML_HEAD_QK)
    kc = to_chunks(k, ML_HEAD_QK) * (ML_HEAD_QK ** -0.5)
    vc = to_chunks(v, ML_HEAD_V)
    ic = gate_chunks(ig)
    lfc = gate_chunks(jax.nn.log_sigmoid(fg))
    init = (jnp.zeros((bsz, ML_HEADS, ML_HEAD_QK, ML_HEAD_V), f32),
            jnp.zeros((bsz, ML_HEADS, ML_HEAD_QK), f32),
            jnp.zeros((bsz, ML_HEADS), f32))
    _, h = lax.scan(_mlstm_chunk_step, init, (qc, kc, vc, ic, lfc))
    h = h.transpose(1, 0, 3, 2, 4).reshape(bsz, seq, ML_HEADS, ML_HEAD_V)
    h = h * lax.rsqrt(jnp.mean(h * h, axis=-1, keepdims=True) + EPS)
    h = (h.reshape(bsz, seq, ML_V_DIM) * head_g.astype(f32)).astype(xn.dtype)
    h = jax.nn.sigmoid(o) * h
    return jnp.einsum('bse,ed->bsd', h, w_out)


def _swiglu(xn, w_gate, w_up, w_down):
    g = jnp.einsum('bsd,df->bsf', xn, w_gate)
    up = jnp.einsum('bsd,df->bsf', xn, w_up)
    return jnp.einsum('bsf,fd->bsd', jax.nn.silu(g) * up, w_down)


def setup_inputs(seed: int = 0) -> dict:
    key = jax.random.key(seed)
    ks = iter(jax.random.split(key, 40))

    def nrm(shape, scale):
        return jax.random.normal(next(ks), shape, jnp.float32) * scale

    def gain(shape):
        return 1.0 + nrm(shape, 0.05)

    lam_u = jax.random.uniform(next(ks), (N_EVEN, LRU_WIDTH), jnp.float32, minval=0.9, maxval=0.999)
    lam_base = lam_u ** (1.0 / LRU_C)
    lru_lambda = jnp.log(lam_base) - jnp.log1p(-lam_base)

    ml_b_in = nrm((N_ODD, ML_IN), 0.02)
    f_bias = jnp.linspace(3.0, 6.0, ML_HEADS, dtype=jnp.float32) + nrm((N_ODD, ML_HEADS), 0.1)
    ml_b_in = ml_b_in.at[:, ML_IN - ML_HEADS:].set(f_bias)

    return {
        'x': nrm((BATCH, SEQ, D_MODEL), 1.0),
        'pre_mix_g': gain((DEPTH, D_MODEL)),
        'post_mix_g': gain((DEPTH, D_MODEL)),
        'pre_ffn_g': gain((DEPTH, D_MODEL)),
        'post_ffn_g': gain((DEPTH, D_MODEL)),
        'ab_w_in': nrm((N_EVEN, D_MODEL, AB_IN), D_MODEL ** -0.5),
        'ab_b_in': nrm((N_EVEN, AB_IN), 0.02),
        'conf_conv_w': nrm((N_EVEN, CONF_KERNEL, CONF_WIDTH), CONF_KERNEL ** -0.5),
        'conf_conv_b': nrm((N_EVEN, CONF_WIDTH), 0.02),
        'conf_ln_g': gain((N_EVEN, CONF_WIDTH)),
        'conf_ln_b': nrm((N_EVEN, CONF_WIDTH), 0.02),
        'lru_conv_w': nrm((N_EVEN, LRU_CONV, LRU_WIDTH), LRU_CONV ** -0.5),
        'lru_conv_b': nrm((N_EVEN, LRU_WIDTH), 0.02),
        'lru_w_a': nrm((N_EVEN, LRU_HEADS, LRU_HEAD_DIM, LRU_HEAD_DIM), LRU_HEAD_DIM ** -0.5),
        'lru_b_a': nrm((N_EVEN, LRU_HEADS, LRU_HEAD_DIM), 0.02),
        'lru_w_x': nrm((N_EVEN, LRU_HEADS, LRU_HEAD_DIM, LRU_HEAD_DIM), LRU_HEAD_DIM ** -0.5),
        'lru_b_x': nrm((N_EVEN, LRU_HEADS, LRU_HEAD_DIM), 0.02),
        'lru_lambda': lru_lambda,
        'ab_w_out': nrm((N_EVEN, AB_OUT, D_MODEL), AB_OUT ** -0.5),
        'ml_w_in': nrm((N_ODD, D_MODEL, ML_IN), D_MODEL ** -0.5),
        'ml_b_in': ml_b_in,
        'ml_head_g': gain((N_ODD, ML_V_DIM)),
        'ml_w_out': nrm((N_ODD, ML_V_DIM, D_MODEL), ML_V_DIM ** -0.5),
        'ffn_w_gate': nrm((DEPTH, D_MODEL, D_FF), D_MODEL ** -0.5),
        'ffn_w_up': nrm((DEPTH, D_MODEL, D_FF), D_MODEL ** -0.5),
        'ffn_w_down': nrm((DEPTH, D_FF, D_MODEL), D_FF ** -0.5),
    }


def reference(x, pre_mix_g, post_mix_g, pre_ffn_g, post_ffn_g,
              ab_w_in, ab_b_in, conf_conv_w, conf_conv_b, conf_ln_g, conf_ln_b,
              lru_conv_w, lru_conv_b, lru_w_a, lru_b_a, lru_w_x, lru_b_x, lru_lambda, ab_w_out,
              ml_w_in, ml_b_in, ml_head_g, ml_w_out,
              ffn_w_gate, ffn_w_up, ffn_w_down):
    for layer in range(DEPTH):
        j = layer // 2
        h = _rmsnorm(x, pre_mix_g[layer])
        if layer % 2 == 0:
            h = _ab_mixer(h, ab_w_in[j], ab_b_in[j], conf_conv_w[j], conf_conv_b[j],
                          conf_ln_g[j], conf_ln_b[j], lru_conv_w[j], lru_conv_b[j],
                          lru_w_a[j], lru_b_a[j], lru_w_x[j], lru_b_x[j], lru_lambda[j], ab_w_out[j])
        else:
            h = _mlstm_mixer(h, ml_w_in[j], ml_b_in[j], ml_head_g[j], ml_w_out[j])
        x = x + _rmsnorm(h, post_mix_g[layer])
        h = _swiglu(_rmsnorm(x, pre_ffn_g[layer]), ffn_w_gate[layer], ffn_w_up[layer], ffn_w_down[layer])
        x = x + _rmsnorm(h, post_ffn_g[layer])
    return x
```

```python
import math
from contextlib import ExitStack
import numpy as np
import concourse.bass as bass
import concourse.mybir as mybir
from concourse.bass_utils import run_bass_kernel_spmd

F32 = mybir.dt.float32
BF16 = mybir.dt.bfloat16
AF = mybir.ActivationFunctionType
ALU = mybir.AluOpType

NCORES = 8
D = 1024
SEQ = 2048
NSEQ = 2
DFF = 2816
NF = DFF // 128
EPS = 1e-6
ML_IN = 3088
import os
ML_STOP = int(os.environ.get('ML_STOP', '0'))
NOIL = int(os.environ.get('NOIL', '0'))
AB_W1, AB_W2, AB_WB, AB_WA, AB_DA = [int(v) for v in os.environ.get('AB_IL', '1,1,1,1,0').split(',')]
ML_WB, ML_WA, ML_DA = [int(v) for v in os.environ.get('ML_IL', '1,1,0').split(',')]


class Sched:
    ENG = ("pe", "act", "dve", "pool", "sp")

    def __init__(self, nc, es):
        self.nc = nc
        self.es = es
        self.eng = {"pe": nc.tensor, "act": nc.scalar, "dve": nc.vector, "pool": nc.gpsimd, "sp": nc.sync}
        self.sem = {}
        self.mult = {}
        self.cnt = {}
        self.clock = {e: {} for e in self.ENG}
        self.snap = {}
        self.lastw = {}
        self.readers = {}
        for e in ("pe", "act", "dve", "pool"):
            self._chan(e, 1)

    def _chan(self, name, mult):
        if name not in self.sem:
            self.sem[name] = self.es.enter_context(self.nc.semaphore("s_" + name))
            self.mult[name] = mult
            self.cnt[name] = 0

    def _wait(self, e, ch, idx):
        ck = self.clock[e]
        if ck.get(ch, 0) >= idx:
            return
        self.eng[e].wait_ge(self.sem[ch], idx * self.mult[ch])
        ck[ch] = idx
        sn = self.snap.get((ch, idx))
        if sn:
            for c2, v2 in sn.items():
                if ck.get(c2, 0) < v2:
                    ck[c2] = v2

    def _deps(self, e, reads, writes):
        for k in reads:
            w = self.lastw.get(k)
            if w is not None:
                self._wait(e, w[0], w[1])
        for k in writes:
            w = self.lastw.get(k)
            if w is not None and w[0] != e:
                self._wait(e, w[0], w[1])
            rd = self.readers.get(k)
            if rd:
                for ch, idx in rd.items():
                    if ch != e:
                        self._wait(e, ch, idx)

    def _register(self, ch, idx, reads, writes):
        for k in reads:
            rd = self.readers.setdefault(k, {})
            if rd.get(ch, 0) < idx:
                rd[ch] = idx
        for k in writes:
            self.lastw[k] = (ch, idx)
            self.readers[k] = {}

    def op(self, e, meth, reads, writes, signal=True, **kw):
        self._deps(e, reads, writes)
        inst = getattr(self.eng[e], meth)(**kw)
        idx = self.cnt[e] + 1
        if signal:
            self.cnt[e] = idx
            inst.then_inc(self.sem[e], 1)
            sn = dict(self.clock[e])
            sn[e] = idx - 1
            self.snap[(e, idx)] = sn
        self._register(e, idx, reads, writes)
        return inst

    def dma(self, e, chan, out, in_, reads, writes):
        ch = "d_" + chan
        self._chan(ch, 16)
        self._deps(e, reads, writes)
        inst = self.eng[e].dma_start(out=out, in_=in_)
        self.cnt[ch] += 1
        idx = self.cnt[ch]
        inst.then_inc(self.sem[ch], 16)
        self.snap[(ch, idx)] = dict(self.clock[e])
        self._register(ch, idx, reads, writes)

    def barrier(self):
        for e in self.ENG:
            for ch, c in self.cnt.items():
                if c > 0 and ch != e:
                    self._wait(e, ch, c)
        self.lastw.clear()
        self.readers.clear()

    def final_wait(self, e, chan):
        ch = "d_" + chan
        self._wait(e, ch, self.cnt[ch])

    def mm(self, out, lhsT, rhs, start, stop, reads, writes, sig=None):
        return self.op("pe", "matmul", reads, writes, signal=(stop if sig is None else sig), out=out, lhsT=lhsT,
                       rhs=rhs, start=start, stop=stop)

    def act(self, out, in_, func, reads, writes, bias=None, scale=1.0):
        kw = dict(out=out, in_=in_, func=func, scale=scale)
        if bias is not None:
            kw["bias"] = bias
        return self.op("act", "activation", reads, writes, **kw)

    def tt(self, out, in0, in1, op, reads, writes, e="dve"):
        return self.op(e, "tensor_tensor", reads, writes, out=out, in0=in0, in1=in1, op=op)

    def stt(self, out, in0, scalar, in1, op0, op1, reads, writes, e="dve"):
        return self.op(e, "scalar_tensor_tensor", reads, writes, out=out, in0=in0, scalar=scalar,
                       in1=in1, op0=op0, op1=op1)

    def ts(self, out, in_, scalar, op, reads, writes, e="dve"):
        return self.op(e, "tensor_single_scalar", reads, writes, out=out, in_=in_, scalar=scalar, op=op)

    def cp(self, out, in_, reads, writes, e="dve"):
        return self.op(e, "tensor_copy", reads, writes, out=out, in_=in_)

    def scan(self, out, d0, d1, init, op0, op1, reads, writes):
        return self.op("dve", "tensor_tensor_scan", reads, writes, out=out, data0=d0, data1=d1,
                       initial=init, op0=op0, op1=op1)

    def recip(self, out, in_, reads, writes):
        return self.op("dve", "reciprocal", reads, writes, out=out, in_=in_)

    def memset(self, ap, val, writes, e="dve"):
        return self.op(e, "memset", [], writes, ap=ap, constant=val)


def _vec_layout():
    names = [("eps", 1), ("one", 1), ("ln8", 1)]
    for l in range(2):
        names += [("g_pm%d" % l, 8), ("g_qm%d" % l, 8), ("g_pf%d" % l, 8), ("g_qf%d" % l, 8)]
    names += [("ab_b", 16), ("ccw", 124), ("ccb", 4), ("lng", 4), ("lnb", 4), ("lcw", 16), ("lcb", 4),
              ("lba", 4), ("lbx", 4), ("lam", 4), ("ml_bq", 4), ("ml_bk", 4), ("ml_bo", 8), ("ml_hg", 8),
              ("ml_bi", 1), ("ml_bf", 1), ("pairsel", 4)]
    off = {}
    o = 0
    for n, c in names:
        off[n] = (o, c)
        o += c
    return off, o


VOFF, NV = _vec_layout()
NSEL = 8 * 128 + 8 * 128 + 128 + 8


def _fm(v, n):
    return np.ascontiguousarray(np.asarray(v, np.float32).reshape(n, 128).T)


def _host_consts(inp):
    vecs = np.zeros((128, NV), np.float32)

    def put(name, arr):
        o, c = VOFF[name]
        vecs[:arr.shape[0], o:o + c] = arr

    put("eps", np.full((128, 1), EPS, np.float32))
    put("one", np.ones((128, 1), np.float32))
    put("ln8", np.full((128, 1), math.log(0.125), np.float32))
    for l in range(2):
        put("g_pm%d" % l, _fm(inp["pre_mix_g"][l], 8))
        put("g_qm%d" % l, _fm(inp["post_mix_g"][l], 8))
        put("g_pf%d" % l, _fm(inp["pre_ffn_g"][l], 8))
        put("g_qf%d" % l, _fm(inp["post_ffn_g"][l], 8))
    put("ab_b", _fm(inp["ab_b_in"][0], 16))
    ccw = np.asarray(inp["conf_conv_w"][0], np.float32)
    put("ccw", np.ascontiguousarray(ccw.reshape(31, 4, 128).transpose(2, 1, 0).reshape(128, 124)))
    put("ccb", _fm(inp["conf_conv_b"][0], 4))
    put("lng", _fm(inp["conf_ln_g"][0], 4))
    put("lnb", _fm(inp["conf_ln_b"][0], 4))
    lcw = np.asarray(inp["lru_conv_w"][0], np.float32)
    put("lcw", np.ascontiguousarray(lcw.reshape(4, 4, 128).transpose(2, 1, 0).reshape(128, 16)))
    put("lcb", _fm(inp["lru_conv_b"][0], 4))
    put("lba", _fm(np.asarray(inp["lru_b_a"][0]).reshape(-1), 4))
    put("lbx", _fm(np.asarray(inp["lru_b_x"][0]).reshape(-1), 4))
    put("lam", _fm(inp["lru_lambda"][0], 4))
    mb = np.asarray(inp["ml_b_in"][0], np.float32)
    put("ml_bq", _fm(mb[0:512], 4))
    put("ml_bk", _fm(mb[512:1024], 4))
    put("ml_bo", _fm(mb[2048:3072], 8))
    put("ml_hg", _fm(inp["ml_head_g"][0], 8))
    put("ml_bi", mb[3072:3080].reshape(8, 1))
    put("ml_bf", mb[3080:3088].reshape(8, 1))
    ps = np.zeros((8, 4), np.float32)
    for h in range(8):
        ps[h, h // 2] = 1.0
    put("pairsel", ps)

    cm = np.zeros((128, 2, 128), np.float32)
    cm[:, 0, :] = np.eye(128, dtype=np.float32)
    cm[:, 1, :] = np.triu(np.ones((128, 128), np.float32))

    sel = np.zeros((8, NSEL), np.float32)
    for h in range(8):
        c2, hh = h // 2, h % 2
        sel[h, h * 128 + hh * 64: h * 128 + hh * 64 + 64] = 1.0
        sel[h, 1024 + h * 128: 1024 + (h + 1) * 128] = 1.0
        sel[h, 2048 + hh * 64: 2048 + hh * 64 + 64] = 1.0
        sel[h, 2176 + h] = 1.0
    bbc = np.ascontiguousarray(np.broadcast_to(mb[512:2048][None, :], (128, 1536))).astype(np.float32)

    lruw = np.zeros((128, 2, 4, 128), np.float32)
    for gi, nm in enumerate(("lru_w_a", "lru_w_x")):
        w = np.asarray(inp[nm][0], np.float32)
        for h in range(8):
            c, hh = h // 2, h % 2
            lruw[hh * 64:(hh + 1) * 64, gi, c, hh * 64:(hh + 1) * 64] = w[h]
    mgw = np.ascontiguousarray(np.asarray(inp["ml_w_in"][0], np.float32)[:, 3072:3088])
    return dict(vecs=vecs, cm=cm, sel=sel, bbc=bbc, lruw=lruw, mgw=mgw)


def build_program(nseq=NSEQ, seq=SEQ, phases=("ab", "ffn0", "ml", "ffn1")):
    nc = bass.Bass("TRN2", target_bir_lowering=False)
    dr = {}

    def din(name, shape):
        dr[name] = nc.dram_tensor(name, list(shape), F32, kind="ExternalInput").ap()
        return dr[name]

    xT = din("xT", [nseq, D, seq])
    vecs_d = din("vecs", [128, NV])
    cm_d = din("cm", [128, 2, 128])
    sel_d = din("sel", [8, NSEL])
    bbc_d = din("bbc", [128, 1536])
    lruw_d = din("lruw", [128, 2, 4, 128])
    mgw_d = din("mgw", [D, 16])
    abwi = din("ab_w_in", [D, 2048])
    abwo = din("ab_w_out", [D, D])
    mlwi = din("ml_w_in", [D, ML_IN])
    mlwo = din("ml_w_out", [D, D])
    fwg = din("ffn_w_gate", [2, D, DFF])
    fwu = din("ffn_w_up", [2, D, DFF])
    fwd = din("ffn_w_down", [2, DFF, D])
    yT = nc.dram_tensor("yT", [nseq, D, seq], F32, kind="ExternalOutput").ap()

    with ExitStack() as es:
        S = Sched(nc, es)

        uid = [0]

        def sb(name, shape, dt, st=None):
            uid[0] += 1
            return (st or es).enter_context(nc.sbuf_tensor("%s_%d" % (name, uid[0]), list(shape), dt))

        PS = [es.enter_context(nc.psum_tensor("ps%d" % i, [128, 512], F32)) for i in range(8)]

        def pk(i):
            return "ps%d" % i

        X = sb("X", [128, 8, seq], F32)
        VEC = sb("VEC", [128, NV], F32)
        CM = sb("CM", [128, 2, 128], F32)
        IDB = sb("IDB", [128, 128], BF16)
        ONESB = sb("ONESB", [128, 128], BF16)
        AVG512 = sb("AVG512", [128, 128], BF16)
        SELB = sb("SELB", [8, NSEL], BF16)
        ONESROW = sb("ONESROW", [8, 128], F32)
        CL = sb("CL", [128, 8], F32)
        NBF = sb("NBF", [8, 1], F32)

        def V(name, c=None, rows=128):
            o, n = VOFF[name]
            if c is None:
                return VEC[0:rows, o:o + n]
            return VEC[0:rows, o + c:o + c + 1]

        S.dma("sp", "c0", VEC[:], vecs_d, [], ["VEC"])
        S.dma("sp", "c0", CM[:], cm_d, [], ["CM"])
        S.dma("pool", "c1", SELB[:], sel_d, [], ["SELB"])
        S.cp(IDB[:], CM[:, 0, :], ["CM"], ["IDB"])
        S.memset(ONESB[:], 1.0, ["ONESB"])
        S.memset(AVG512[:], 1.0 / 512.0, ["AVG512"])
        S.memset(ONESROW[:], 1.0, ["ONESROW"])
        tmpc = sb("tmpc", [128, 4], F32)
        S.act(tmpc[:], V("lam"), AF.Exp, ["VEC"], ["tmpc"], scale=-1.0)
        S.act(tmpc[:], tmpc[:], AF.Ln, ["tmpc"], ["tmpc"], bias=V("one", 0))
        S.ts(CL[:, 0:4], tmpc[:], -8.0, ALU.mult, ["tmpc"], ["CL"])
        S.ts(CL[:, 4:8], tmpc[:], -16.0, ALU.mult, ["tmpc"], ["CL"])
        S.ts(NBF[:], V("ml_bf", 0, 8), -1.0, ALU.mult, ["VEC"], ["NBF"])
        MASK = CM[:, 1, :]

        def load_x(s):
            for c in range(8):
                S.dma("sp", "xld", X[:, c, :], xT[s, c * 128:(c + 1) * 128, :], [], ["X"])

        def store_x(s):
            for c in range(8):
                S.dma("sp", "xst", yT[s, c * 128:(c + 1) * 128, :], X[:, c, :], ["X"], [])

        def il(*gens):
            gens = list(gens)
            while gens:
                for g in list(gens):
                    try:
                        next(g)
                        yield
                    except StopIteration:
                        gens.remove(g)

        def ilw(specs):
            specs = [[g, w, d] for g, w, d in specs]
            rnd = 0
            while specs:
                for sp in list(specs):
                    if rnd < sp[2]:
                        continue
                    for _ in range(sp[1]):
                        try:
                            next(sp[0])
                            yield
                        except StopIteration:
                            specs.remove(sp)
                            break
                rnd += 1

        def run(g):
            for _ in g:
                pass

        def pipeline(n, stageA, stageB, wB=1, wA=1, dA=0):
            run(stageA(0))
            for i in range(n):
                if i + 1 < n and not NOIL:
                    run(ilw([(stageB(i), wB, 0), (stageA(i + 1), wA, dA)]))
                elif i + 1 < n:
                    run(stageB(i))
                    run(stageA(i + 1))
                else:
                    run(stageB(i))

        def rstd_act(out, in_, scale, reads, wkey):
            S.act(out, in_, AF.Ln, list(reads) + ["VEC"], [wkey], bias=V("eps", 0), scale=scale)
            S.act(out, out, AF.Exp, [wkey], [wkey], scale=-0.5)

        def prenorm(gname, t0, T, XN, SQ, RSTD, xn_key, bank, extra_w=(), xk="X", sqk="SQ", rk="RSTD"):
            S.act(SQ[:, :, 0:T], X[:, :, t0:t0 + T], AF.Square, [xk], [sqk] + list(extra_w))
            for c in range(8):
                S.mm(PS[bank][:, 0:T], ONESB[:], SQ[:, c, 0:T], c == 0, c == 7, [sqk, "ONESB"], [pk(bank)])
            rstd_act(RSTD[:, 0:T], PS[bank][:, 0:T], 1.0 / D, [pk(bank)], rk)
            for c in range(8):
                S.stt(XN[:, c, 0:T], X[:, c, t0:t0 + T], V(gname, c), RSTD[:, 0:T], ALU.mult, ALU.mult,
                      [xk, rk, "VEC"], [xn_key] + list(extra_w))

        def postnorm(gname, t0, T, HM, SQ, RSTD, TMP, bank, hm_key, stats_done=False, xk="X", sqk="SQ",
                     rk="RSTD", tk="TMPN"):
            if not stats_done:
                for c in range(8):
                    S.mm(PS[bank][:, 0:T], ONESB[:], SQ[:, c, 0:T], c == 0, c == 7, [sqk, "ONESB"], [pk(bank)])
            rstd_act(RSTD[:, 0:T], PS[bank][:, 0:T], 1.0 / D, [pk(bank)], rk)
            for c in range(8):
                ti = c % len(TMP)
                S.stt(TMP[ti][:, 0:T], HM[:, c, 0:T], V(gname, c), RSTD[:, 0:T], ALU.mult, ALU.mult,
                      [hm_key, rk, "VEC"], [tk + str(ti)])
                S.tt(X[:, c, t0:t0 + T], X[:, c, t0:t0 + T], TMP[ti][:, 0:T], ALU.add, [xk, tk + str(ti)], [xk])

        def mixer_ab(s):
            T = 256
            NT = seq // T
            with ExitStack() as st:
                WI = sb("WI", [128, 8, 2048], BF16, st)
                WO = sb("WO", [128, 8, 1024], BF16, st)
                LW = sb("LW", [128, 2, 4, 128], BF16, st)
                XN = sb("XN", [128, 8, T], BF16, st)
                SQA = sb("SQA", [128, 8, T], BF16, st)
                SQB = sb("SQB", [128, 8, T], BF16, st)
                RSA = sb("RSA", [128, T], F32, st)
                RSB = sb("RSB", [128, T], F32, st)
                TMPN = [sb("TMPN%d" % i, [128, T], F32, st) for i in range(2)]
                DG = [sb("DG%d" % i, [128, 31, 128], BF16, st) for i in range(2)]
                DGL = sb("DGL", [128, 16, 128], BF16, st)
                CI = [sb("CI%d" % i, [128, 4, 30 + T], BF16, st) for i in range(2)]
                RI = [sb("RI%d" % i, [128, 4, 3 + T], BF16, st) for i in range(2)]
                GG = [sb("GG%d" % i, [128, 4, T], BF16, st) for i in range(2)]
                SG = sb("SG", [128, T], F32, st)
                UY = sb("UY", [128, 8 * T], F32, st)
                HM = UY[:].rearrange("p (c t) -> p c t", c=8)
                YC = UY[:, 0:4 * T].rearrange("p (c t) -> p c t", c=4)
                YC2 = UY[:, 4 * T:6 * T].bitcast(BF16).rearrange("p (c t) -> p c t", c=4)
                YCB = UY[:, 6 * T:8 * T].bitcast(BF16).rearrange("p (c t) -> p c t", c=4)
                LN1 = sb("LN1", [128, T], F32, st)
                LN2 = sb("LN2", [128, T], F32, st)
                ZT = [sb("ZT%d" % i, [128, T], F32, st) for i in range(2)]
                YCAT = sb("YCAT", [128, 8, T], BF16, st)
                XR = sb("XR", [128, 4, T], F32, st)
                XRB = sb("XRB", [128, 4, T], BF16, st)
                LR = [sb("LR%d" % i, [128, T], F32, st) for i in range(4)]
                LI = [sb("LI%d" % i, [128, T], F32, st) for i in range(4)]
                LS = [sb("LS%d" % i, [128, T], F32, st) for i in range(4)]
                HH = [sb("HH%d" % i, [128, T], F32, st) for i in range(2)]
                HST = sb("HST", [128, 4], F32, st)
                print("ab sbuf remaining", nc.sbuf_bytes_remaining)

                for k in range(8):
                    S.dma("pool", "wmi", WI[:, k, :], abwi[k * 128:(k + 1) * 128, :], [], ["WI"])
                for k in range(8):
                    S.dma("pool", "wmo", WO[:, k, :], abwo[k * 128:(k + 1) * 128, :], [], ["WO"])
                S.dma("pool", "lw", LW[:], lruw_d, [], ["LW"])
                for j in range(16):
                    S.ts(DGL[:, j, :], IDB[:], V("lcw", j), ALU.mult, ["IDB", "VEC"], ["DGL"], e="pool")
                S.memset(HST[:], 0.0, ["HST"])
                o_ccw = VOFF["ccw"][0]

                def stageA(i):
                    par = i % 2
                    t0 = i * T
                    ci, ri, gg = CI[par], RI[par], GG[par]
                    kci, kri, kgg = "CI%d" % par, "RI%d" % par, "GG%d" % par
                    if i == 0:
                        S.memset(ci[:, :, 0:30], 0.0, [kci])
                        S.memset(ri[:, :, 0:3], 0.0, [kri])
                    else:
                        S.cp(ci[:, :, 0:30], CI[1 - par][:, :, T:T + 30], ["CI%d" % (1 - par)], [kci], e="pool")
                        S.cp(ri[:, :, 0:3], RI[1 - par][:, :, T:T + 3], ["RI%d" % (1 - par)], [kri], e="pool")
                    yield
                    prenorm("g_pm0", t0, T, XN, SQA, RSA, "XN", 2, xk="X%d" % i, sqk="SQA", rk="RSA")
                    yield
                    bankc = [0]

                    def proj(e):
                        b = bankc[0] % 3
                        bankc[0] += 1
                        for k in range(8):
                            S.mm(PS[b][:, 0:T], WI[:, k, e * 128:(e + 1) * 128], XN[:, k, :], k == 0, k == 7,
                                 ["WI", "XN"], [pk(b)])
                        return b

                    for c in range(4):
                        bg = proj(4 + c)
                        S.act(SG[:], PS[bg][:, 0:T], AF.Sigmoid, [pk(bg), "VEC"], ["SG"], bias=V("ab_b", 4 + c))
                        bv = proj(c)
                        S.stt(ci[:, c, 30:30 + T], PS[bv][:, 0:T], V("ab_b", c), SG[:], ALU.add, ALU.mult,
                              [pk(bv), "SG", "VEC"], [kci])
                        yield
                    for c in range(4):
                        b = proj(8 + c)
                        S.act(gg[:, c, :], PS[b][:, 0:T], AF.Gelu, [pk(b), "VEC"], [kgg], bias=V("ab_b", 8 + c))
                        yield
                    for c in range(4):
                        b = proj(12 + c)
                        S.act(ri[:, c, 3:3 + T], PS[b][:, 0:T], AF.Identity, [pk(b), "VEC"], [kri],
                              bias=V("ab_b", 12 + c))
                        yield

                def stageB1(i):
                    par = i % 2
                    ci, kci = CI[par], "CI%d" % par
                    for c in range(4):
                        wv = VEC[:, o_ccw + c * 31:o_ccw + (c + 1) * 31]
                        dg, kdg = DG[c % 2], "DG%d" % (c % 2)
                        S.tt(dg[:], IDB[:].unsqueeze(1).broadcast_to([128, 31, 128]),
                             wv.unsqueeze(2).broadcast_to([128, 31, 128]), ALU.mult, ["IDB", "VEC"], [kdg],
                             e=("pool" if c % 2 == 0 else "dve"))
                        if c == 0:
                            yield
                        for k in range(31):
                            S.mm(PS[3][:, 0:T], dg[:, k, :], ci[:, c, k:k + T], k == 0, k == 30,
                                 [kdg, kci], [pk(3)])
                        S.act(YC[:, c, :], PS[3][:, 0:T], AF.Identity, [pk(3), "VEC"], ["YC", "HM"], bias=V("ccb", c))
                        S.act(YC2[:, c, :], PS[3][:, 0:T], AF.Square, [pk(3), "VEC"], ["YC2", "HM"], bias=V("ccb", c))
                        S.cp(YCB[:, c, :], YC[:, c, :], ["YC"], ["YCB", "HM"], e="pool")
                        yield
                    for c in range(4):
                        S.mm(PS[4][:, 0:T], AVG512[:], YCB[:, c, :], c == 0, c == 3, ["YCB", "AVG512"], [pk(4)])
                    for c in range(4):
                        S.mm(PS[5][:, 0:T], AVG512[:], YC2[:, c, :], c == 0, c == 3, ["YC2", "AVG512"], [pk(5)])
                    yield
                    S.act(LN1[:], PS[4][:, 0:T], AF.Square, [pk(4)], ["LN1"])
                    S.tt(LN1[:], PS[5][:, 0:T], LN1[:], ALU.subtract, [pk(5), "LN1"], ["LN1"])
                    S.ts(LN1[:], LN1[:], 0.0, ALU.max, ["LN1"], ["LN1"])
                    yield
                    rstd_act(LN2[:], LN1[:], 1.0, ["LN1"], "LN2")
                    yield
                    for c in range(4):
                        zt, kz = ZT[c % 2], "ZT%d" % (c % 2)
                        S.tt(zt[:], YC[:, c, :], PS[4][:, 0:T], ALU.subtract, ["YC", pk(4)], [kz])
                        S.tt(zt[:], zt[:], LN2[:], ALU.mult, [kz, "LN2"], [kz])
                        S.act(YCAT[:, c, :], zt[:], AF.Silu, [kz, "VEC"], ["YCATa"], bias=V("lnb", c),
                              scale=V("lng", c))
                        yield

                def stageB2(i):
                    par = i % 2
                    ri, gg = RI[par], GG[par]
                    kri, kgg = "RI%d" % par, "GG%d" % par
                    for c in range(4):
                        for k in range(4):
                            S.mm(PS[6][:, 0:T], DGL[:, c * 4 + k, :], ri[:, c, k:k + T], k == 0, k == 3,
                                 ["DGL", kri], [pk(6)])
                        S.act(XR[:, c, :], PS[6][:, 0:T], AF.Identity, [pk(6), "VEC"], ["XR%d" % c], bias=V("lcb", c))
                        S.cp(XRB[:, c, :], XR[:, c, :], ["XR%d" % c], ["XRB%d" % c], e="pool")
                        yield
                    for hb in range(2):
                        cs = (2 * hb, 2 * hb + 1)
                        for j, c in enumerate(cs):
                            S.mm(PS[6][:, j * T:(j + 1) * T], LW[:, 0, c, :], XRB[:, c, :], True, True,
                                 ["LW", "XRB%d" % c], [pk(6)])
                            S.mm(PS[7][:, j * T:(j + 1) * T], LW[:, 1, c, :], XRB[:, c, :], True, True,
                                 ["LW", "XRB%d" % c], [pk(7)])
                        yield
                        for j, c in enumerate(cs):
                            S.act(LR[c][:], PS[6][:, j * T:(j + 1) * T], AF.Sigmoid, [pk(6), "VEC"], ["LR%d" % c],
                                  bias=V("lba", c))
                            S.act(LI[c][:], PS[7][:, j * T:(j + 1) * T], AF.Sigmoid, [pk(7), "VEC"], ["LI%d" % c],
                                  bias=V("lbx", c))
                        yield
                        for c in cs:
                            S.act(LS[c][:], LR[c][:], AF.Exp, ["LR%d" % c, "CL"], ["LS%d" % c], scale=CL[:, 4 + c:5 + c])
                            S.act(LR[c][:], LR[c][:], AF.Exp, ["LR%d" % c, "CL"], ["LR%d" % c], scale=CL[:, c:c + 1])
                            S.tt(LI[c][:], LI[c][:], XR[:, c, :], ALU.mult, ["LI%d" % c, "XR%d" % c], ["LI%d" % c])
                        yield
                        for c in cs:
                            S.op("dve", "tensor_scalar", ["LS%d" % c], ["LS%d" % c], out=LS[c][:], in0=LS[c][:],
                                 scalar1=-1.0, scalar2=1.0, op0=ALU.mult, op1=ALU.add)
                            S.ts(LS[c][:], LS[c][:], 1e-30, ALU.max, ["LS%d" % c], ["LS%d" % c])
                        yield
                        for c in cs:
                            S.act(LS[c][:], LS[c][:], AF.Ln, ["LS%d" % c], ["LS%d" % c])
                        for c in cs:
                            S.act(LS[c][:], LS[c][:], AF.Exp, ["LS%d" % c], ["LS%d" % c], scale=0.5)
                        yield
                        for j, c in enumerate(cs):
                            S.tt(LI[c][:], LI[c][:], LS[c][:], ALU.mult, ["LI%d" % c, "LS%d" % c], ["LI%d" % c])
                            S.scan(HH[j][:], LR[c][:], LI[c][:], HST[:, c:c + 1], ALU.mult, ALU.add,
                                   ["LR%d" % c, "LI%d" % c, "HST"], ["HH%d" % j])
                        yield
                        for j, c in enumerate(cs):
                            S.cp(HST[:, c:c + 1], HH[j][:, T - 1:T], ["HH%d" % j], ["HST"])
                            S.tt(YCAT[:, 4 + c, :], HH[j][:], gg[:, c, :], ALU.mult, ["HH%d" % j, kgg], ["YCATb"])
                        yield

                def stageB3(i):
                    t0 = i * T
                    for d in range(8):
                        b = 3 + d % 2
                        for k in range(8):
                            S.mm(PS[b][:, 0:T], WO[:, k, d * 128:(d + 1) * 128], YCAT[:, k, :], k == 0, k == 7,
                                 ["WO", "YCATa", "YCATb"], [pk(b)])
                        S.act(HM[:, d, :], PS[b][:, 0:T], AF.Identity, [pk(b)], ["HM", "YC", "YC2", "YCB"])
                        S.act(SQB[:, d, :], PS[b][:, 0:T], AF.Square, [pk(b)], ["SQB"])
                        yield
                    postnorm("g_qm0", t0, T, HM, SQB, RSB, TMPN, 5, "HM", xk="X%d" % i, sqk="SQB", rk="RSB")
                    yield

                def stageB(i):
                    yield from ilw([(stageB2(i), AB_W2, 0), (stageB1(i), AB_W1, 0)])
                    yield from stageB3(i)

                pipeline(NT, stageA, stageB, AB_WB, AB_WA, AB_DA)
                S.barrier()

        def ffn(l):
            TP = 1024
            NH = TP // 512
            NP = seq // TP
            with ExitStack() as st:
                HM = sb("FHM", [128, 8, TP], F32, st)
                XN = sb("FXN", [128, 8, TP], BF16, st)
                RSTD = sb("FRSTD", [128, 512], F32, st)
                TMPN = [sb("FTMPN", [128, 512], F32, st)]
                ACTB = sb("ACTB", [128, NF, TP], BF16, st)
                WGU = [sb("WGU%d" % i, [128, 2, 8, 256], BF16, st) for i in range(2)]
                WD = [sb("WD%d" % i, [128, NF, 128], BF16, st) for i in range(3)]
                SGF = [sb("SGF%d" % i, [128, 512], F32, st) for i in range(2)]
                SQT = [sb("SQT%d" % i, [128, 512], BF16, st) for i in range(2)]
                print("ffn sbuf remaining", nc.sbuf_bytes_remaining)
                wg_v = fwg[l].rearrange("(k p) f -> p k f", p=128)
                wu_v = fwu[l].rearrange("(k p) f -> p k f", p=128)
                wd_v = fwd[l].rearrange("(f p) d -> p f d", p=128)
                sqc = [0]

                def ld_gu(j):
                    i = j % 2
                    S.dma("pool", "wgu%d" % i, WGU[i][:, 0, :, :], wg_v[:, :, j * 256:(j + 1) * 256], [], ["WGU%d" % i])
                    S.dma("pool", "wgu%d" % i, WGU[i][:, 1, :, :], wu_v[:, :, j * 256:(j + 1) * 256], [], ["WGU%d" % i])

                def ld_d(d):
                    i = d % 3
                    S.dma("pool", "wd%d" % i, WD[i][:], wd_v[:, :, d * 128:(d + 1) * 128], [], ["WD%d" % i])

                def fprenorm(p, banks):
                    for h2 in range(NH):
                        t0 = p * TP + h2 * 512
                        bank = banks[h2]
                        for c in range(8):
                            sqt = SQT[sqc[0] % 2]
                            qk = "SQT%d" % (sqc[0] % 2)
                            sqc[0] += 1
                            S.act(sqt[:], X[:, c, t0:t0 + 512], AF.Square, ["X"], [qk])
                            S.mm(PS[bank][:], ONESB[:], sqt[:], c == 0, c == 7, [qk, "ONESB"], [pk(bank)], sig=True)
                        rstd_act(RSTD[:], PS[bank][:], 1.0 / D, [pk(bank)], "RSTD")
                        for c in range(8):
                            S.stt(XN[:, c, h2 * 512:(h2 + 1) * 512], X[:, c, t0:t0 + 512], V("g_pf%d" % l, c), RSTD[:],
                                  ALU.mult, ALU.mult, ["X", "RSTD", "VEC"], ["FXN"])

                ld_gu(0)
                ld_gu(1)
                fprenorm(0, (6, 7))
                for p in range(NP):
                    p0 = p * TP
                    cnt = 0
                    for j in range(NF // 2):
                        i = j % 2
                        for fc in range(2):
                            f = 2 * j + fc
                            for h2 in range(NH):
                                bg = (cnt % 2) * 2
                                bu = bg + 1
                                sgf = SGF[cnt % 2]
                                sk = "SGF%d" % (cnt % 2)
                                cnt += 1
                                for k in range(8):
                                    S.mm(PS[bg][:], WGU[i][:, 0, k, fc * 128:(fc + 1) * 128],
                                         XN[:, k, h2 * 512:(h2 + 1) * 512], k == 0, k == 7,
                                         ["WGU%d" % i, "FXN"], [pk(bg)])
                                for k in range(8):
                                    S.mm(PS[bu][:], WGU[i][:, 1, k, fc * 128:(fc + 1) * 128],
                                         XN[:, k, h2 * 512:(h2 + 1) * 512], k == 0, k == 7,
                                         ["WGU%d" % i, "FXN"], [pk(bu)])
                                S.act(sgf[:], PS[bg][:], AF.Silu, [pk(bg)], [sk])
                                S.tt(ACTB[:, f, h2 * 512:(h2 + 1) * 512], sgf[:], PS[bu][:], ALU.mult,
                                     [sk, pk(bu)], ["ACTB"])
                        if j + 2 < NF // 2:
                            ld_gu(j + 2)
                        if j == 0:
                            ld_d(0)
                            ld_d(1)
                            ld_d(2)
                    if p + 1 < NP:
                        fprenorm(p + 1, (0, 1))
                    pend = []
                    for d in range(8):
                        i = d % 3
                        for h2 in range(NH):
                            b = 4 + h2
                            for f in range(NF):
                                S.mm(PS[b][:], WD[i][:, f, :], ACTB[:, f, h2 * 512:(h2 + 1) * 512], f == 0, f == NF - 1,
                                     ["WD%d" % i, "ACTB"], [pk(b)])
                            for (psq, pqk, ph2, pd) in pend:
                                S.mm(PS[6 + ph2][:], ONESB[:], psq[:], pd == 0, pd == 7, [pqk, "ONESB"],
                                     [pk(6 + ph2)], sig=True)
                            pend = []
                            sqt = SQT[sqc[0] % 2]
                            qk = "SQT%d" % (sqc[0] % 2)
                            sqc[0] += 1
                            S.act(sqt[:], PS[b][:], AF.Square, [pk(b)], [qk])
                            S.act(HM[:, d, h2 * 512:(h2 + 1) * 512], PS[b][:], AF.Identity, [pk(b)], ["HM"])
                            pend.append((sqt, qk, h2, d))
                        if d + 3 < 8:
                            ld_d(d + 3)
                        if d == 1 and p + 1 < NP:
                            ld_gu(0)
                            ld_gu(1)
                    for (psq, pqk, ph2, pd) in pend:
                        S.mm(PS[6 + ph2][:], ONESB[:], psq[:], pd == 0, pd == 7, [pqk, "ONESB"], [pk(6 + ph2)], sig=True)
                    for h2 in range(NH):
                        postnorm("g_qf%d" % l, p0 + h2 * 512, 512, HM[:, :, h2 * 512:(h2 + 1) * 512], None, RSTD,
                                 TMPN, 6 + h2, "HM", stats_done=True)
                S.barrier()

        def mixer_ml(s):
            T = 128
            NT = seq // T
            with ExitStack() as st:
                WI = sb("MWI", [128, 8, 3072], BF16, st)
                WO = sb("MWO", [128, 8, 1024], BF16, st)
                WGm = sb("MWG", [128, 8, 16], BF16, st)
                BBC = sb("BBC", [128, 1536], F32, st)
                XN = sb("MXN", [128, 8, T], BF16, st)
                SQA = sb("MSQA", [128, 8, T], BF16, st)
                SQB = sb("MSQB", [128, 8, T], BF16, st)
                RSA = sb("MRSA", [128, T], F32, st)
                RSB = sb("MRSB", [128, T], F32, st)
                TMPN = [sb("MTMPN%d" % i, [128, T], F32, st) for i in range(2)]
                FI = sb("FI", [8, T], F32, st)
                SPt = sb("SPt", [8, T], F32, st)
                Gr = sb("Gr", [8, T], F32, st)
                Ar = sb("Ar", [8, T], F32, st)
                Pr = sb("Pr", [8, T], F32, st)
                Mr = sb("Mr", [8, T], F32, st)
                E8 = sb("E8", [8, T], BF16, st)
                Wr = sb("Wr", [8, T], BF16, st)
                EMIN = sb("EMIN", [8, T], BF16, st)
                GST = sb("GST", [8, 1], F32, st)
                PST = sb("PST", [8, 1], F32, st)
                NPC = sb("NPC", [8, 1], F32, st)
                PCL = sb("PCL", [8, 1], F32, st)
                DEC = sb("DEC", [8, 1], F32, st)
                DR = sb("DR", [8, 4], BF16, st)
                EBs = sb("EBs", [128, 8, T], F32, st)
                KTt = sb("KTt", [128, 512], F32, st)
                EMBs = [sb("EMBs%d" % i, [128, 8, T], F32, st) for i in range(2)]
                WTs = [sb("WTs%d" % i, [128, 8], F32, st) for i in range(2)]
                DBs = [sb("DBs%d" % i, [128, 4], F32, st) for i in range(2)]
                QP = [sb("QP%d" % i, [128, 8, T], BF16, st) for i in range(2)]
                KF = [sb("KF%d" % i, [128, 4, T], BF16, st) for i in range(2)]
                KP = [sb("KP%d" % i, [128, 512], BF16, st) for i in range(2)]
                VT = [sb("VT%d" % i, [128, 1024], BF16, st) for i in range(2)]
                OG = [sb("OG%d" % i, [128, 8, T], BF16, st) for i in range(2)]
                CS = sb("CS", [128, 4, 129], F32, st)
                CB = sb("CB", [128, 4, 128], BF16, st)
                NB = sb("NB", [128, 4, 128], BF16, st)
                SD = sb("SD", [128, 8, T], BF16, st)
                DEN = sb("DEN", [128, 8, T], F32, st)
                HT = sb("HT", [128, 8, T], F32, st)
                SQ2 = sb("SQ2", [128, 8, T], BF16, st)
                HB = sb("HB", [128, 8, T], BF16, st)
                HM = sb("MHM", [128, 8, T], F32, st)
                print("ml sbuf remaining", nc.sbuf_bytes_remaining)

                for k in range(8):
                    S.dma("pool", "wmi", WI[:, k, :], mlwi[k * 128:(k + 1) * 128, 0:3072], [], ["MWI"])
                for k in range(8):
                    S.dma("pool", "wmo", WO[:, k, :], mlwo[k * 128:(k + 1) * 128, :], [], ["MWO"])
                S.dma("pool", "lw", WGm[:], mgw_d.rearrange("(k p) e -> p k e", p=128), [], ["MWG"])
                S.dma("sp", "c0", BBC[:], bbc_d, [], ["BBC"])
                S.memset(CS[:], 0.0, ["CS"])
                S.memset(GST[:], 0.0, ["GST"])
                S.memset(PST[:], 0.0, ["PST"])
                SELPH = SELB[:, 0:1024].rearrange("p (c q) -> p c q", c=8)
                SELH = SELB[:, 1024:2048].rearrange("p (c q) -> p c q", c=8)
                SELODD = SELB[:, 2048:2176]
                I8 = SELB[:, 2176:2184]

                def flat(ap):
                    return ap.rearrange("p c t -> p (c t)")

                def gates(i):
                    par = i % 2
                    kE, kW, kD = "EMBs%d" % par, "WTs%d" % par, "DBs%d" % par
                    for k in range(8):
                        S.mm(PS[0][0:8, 0:T], WGm[:, k, 0:8], XN[:, k, :], k == 0, k == 7, ["MWG", "MXN"], [pk(0)])
                    for k in range(8):
                        S.mm(PS[0][0:8, 128:128 + T], WGm[:, k, 8:16], XN[:, k, :], k == 0, k == 7,
                             ["MWG", "MXN"], [pk(0)])
                    yield
                    S.act(FI[:], PS[0][0:8, 0:T], AF.Identity, [pk(0), "VEC"], ["FI"], bias=V("ml_bi", 0, 8))
                    S.act(SPt[:], PS[0][0:8, 128:128 + T], AF.Exp, [pk(0), "NBF"], ["SPt"], bias=NBF[:], scale=-1.0)
                    S.act(SPt[:], SPt[:], AF.Ln, ["SPt", "VEC"], ["SPt"], bias=V("one", 0, 8))
                    yield
                    S.scan(Gr[:], ONESROW[:, 0:T], SPt[:], GST[:], ALU.mult, ALU.subtract,
                           ["ONESROW", "SPt", "GST"], ["Gr"])
                    yield
                    S.cp(GST[:], Gr[:, T - 1:T], ["Gr"], ["GST"])
                    S.tt(Ar[:], FI[:], Gr[:], ALU.subtract, ["FI", "Gr"], ["Ar"])
                    yield
                    S.scan(Pr[:], Ar[:], Ar[:], PST[:], ALU.max, ALU.max, ["Ar", "PST"], ["Pr"])
                    yield
                    S.ts(NPC[:], Pr[:, T - 1:T], -1.0, ALU.mult, ["Pr"], ["NPC"])
                    S.ts(PCL[:], Pr[:, T - 1:T], math.log(0.125), ALU.add, ["Pr"], ["PCL"])
                    S.tt(Mr[:], Gr[:], Pr[:], ALU.add, ["Gr", "Pr"], ["Mr"])
                    yield
                    S.act(DEC[:], PST[:], AF.Exp, ["PST", "NPC"], ["DEC"], bias=NPC[:])
                    S.act(E8[:], Pr[:], AF.Exp, ["Pr", "PCL"], ["E8"], bias=PCL[:], scale=-1.0)
                    S.act(Wr[:], Ar[:], AF.Exp, ["Ar", "NPC"], ["Wr"], bias=NPC[:])
                    S.act(EMIN[:], Mr[:], AF.Exp, ["Mr"], ["EMIN"], scale=-1.0)
                    yield
                    S.cp(PST[:], Pr[:, T - 1:T], ["Pr"], ["PST"])
                    S.ts(DR[:], V("pairsel", None, 8), DEC[:], ALU.mult, ["VEC", "DEC"], ["DR"])
                    yield
                    S.mm(PS[0][:, 0:8], Wr[:], I8, True, True, ["Wr", "SELB"], [pk(0)])
                    S.mm(PS[0][:, 8:12], SELODD, DR[:], True, True, ["DR", "SELB"], [pk(0)])
                    yield
                    S.cp(WTs[par][:], PS[0][:, 0:8], [pk(0)], [kW])
                    S.cp(DBs[par][:], PS[0][:, 8:12], [pk(0)], [kD])
                    yield
                    for g in range(2):
                        for h in range(4 * g, 4 * g + 4):
                            S.mm(PS[1][:, (h % 4) * 128:(h % 4 + 1) * 128], SELPH[:, h, :], E8[:], True, True,
                                 ["SELB", "E8"], [pk(1)])
                        for h in range(4 * g, 4 * g + 4):
                            S.mm(PS[0][:, (h % 4) * 128:(h % 4 + 1) * 128], SELH[:, h, :], EMIN[:], True, True,
                                 ["SELB", "EMIN"], [pk(0)])
                        yield
                        S.act(flat(EBs[:, 4 * g:4 * g + 4, :]), PS[1][:], AF.Identity, [pk(1)], ["EBs"])
                        S.cp(flat(EMBs[par][:, 4 * g:4 * g + 4, :]), PS[0][:], [pk(0)], [kE])
                        yield

                def kvo(i):
                    par = i % 2
                    kf, vt, og = KF[par], VT[par], OG[par]
                    kkf, kvt, kog = "KF%d" % par, "VT%d" % par, "OG%d" % par
                    for hv in range(2):
                        for k in range(8):
                            S.mm(PS[2 + hv][:], XN[:, k, :], WI[:, k, 1024 + hv * 512:1024 + (hv + 1) * 512],
                                 k == 0, k == 7, ["MWI", "MXN"], [pk(2 + hv)])
                        S.tt(vt[:, hv * 512:(hv + 1) * 512], PS[2 + hv][:], BBC[:, 512 + hv * 512:512 + (hv + 1) * 512],
                             ALU.add, [pk(2 + hv), "BBC"], [kvt])
                        yield
                    for k in range(8):
                        S.mm(PS[2][:], XN[:, k, :], WI[:, k, 512:1024], k == 0, k == 7, ["MWI", "MXN"], [pk(2)])
                    S.tt(KTt[:], PS[2][:], BBC[:, 0:512], ALU.add, [pk(2), "BBC"], ["KTt"])
                    yield
                    for c2 in range(4):
                        for k in range(8):
                            S.mm(PS[3][:, c2 * 128:(c2 + 1) * 128], WI[:, k, 512 + c2 * 128:512 + (c2 + 1) * 128],
                                 XN[:, k, :], k == 0, k == 7, ["MWI", "MXN"], [pk(3)])
                        yield
                    for c2 in range(4):
                        S.act(kf[:, c2, :], PS[3][:, c2 * 128:(c2 + 1) * 128], AF.Identity, [pk(3), "VEC"], [kkf],
                              bias=V("ml_bk", c2))
                    yield
                    for hc in range(8):
                        b = 2 + hc // 4
                        for k in range(8):
                            S.mm(PS[b][:, (hc % 4) * 128:(hc % 4 + 1) * 128],
                                 WI[:, k, 2048 + hc * 128:2048 + (hc + 1) * 128], XN[:, k, :], k == 0, k == 7,
                                 ["MWI", "MXN"], [pk(b)])
                        yield
                    for hc in range(8):
                        b = 2 + hc // 4
                        S.act(og[:, hc, :], PS[b][:, (hc % 4) * 128:(hc % 4 + 1) * 128], AF.Sigmoid,
                              [pk(b), "VEC"], [kog], bias=V("ml_bo", hc))
                        if hc % 4 == 3:
                            yield

                def stageA(i):
                    par = i % 2
                    t0 = i * T
                    prenorm("g_pm1", t0, T, XN, SQA, RSA, "MXN", 3, xk="X%d" % i, sqk="MSQA", rk="MRSA")
                    yield
                    yield from il(gates(i), kvo(i))
                    S.tt(KP[par][:].rearrange("p (h d) -> p h d", h=8), KTt[:].rearrange("p (h d) -> p h d", h=8),
                         WTs[par][:].unsqueeze(2).broadcast_to([128, 8, 64]), ALU.mult, ["KTt", "WTs%d" % par],
                         ["KP%d" % par])
                    for c2 in range(4):
                        for k in range(8):
                            S.mm(PS[2][:, c2 * 128:(c2 + 1) * 128], WI[:, k, c2 * 128:(c2 + 1) * 128], XN[:, k, :],
                                 k == 0, k == 7, ["MWI", "MXN"], [pk(2)])
                        yield
                    for h in range(8):
                        c2 = h // 2
                        S.stt(QP[par][:, h, :], PS[2][:, c2 * 128:(c2 + 1) * 128], V("ml_bq", c2), EBs[:, h, :],
                              ALU.add, ALU.mult, [pk(2), "EBs", "VEC"], ["QP%d" % par])
                        if h % 4 == 3:
                            yield

                def stageB(i):
                    par = i % 2
                    t0 = i * T
                    qp, kf, kp, vt, og = QP[par], KF[par], KP[par], VT[par], OG[par]
                    kqp, kkf, kkp, kvt, kog = "QP%d" % par, "KF%d" % par, "KP%d" % par, "VT%d" % par, "OG%d" % par
                    embs, wts, dbs = EMBs[par], WTs[par], DBs[par]
                    kE, kW, kD = "EMBs%d" % par, "WTs%d" % par, "DBs%d" % par
                    for c2 in range(4):
                        S.ts(CS[:, c2, :], CS[:, c2, :], dbs[:, c2:c2 + 1], ALU.mult, ["CS", kD], ["CS"])
                    S.cp(CB[:], CS[:, :, 0:128], ["CS"], ["CB"])
                    S.cp(NB[:], CS[:, :, 128:129].broadcast_to([128, 4, 128]), ["CS"], ["NB"])
                    yield
                    for h in range(8):
                        b = 4 + h // 4
                        S.mm(PS[b][:, (h % 4) * 128:(h % 4 + 1) * 128], kf[:, h // 2, :], qp[:, h, :], True, True,
                             [kkf, kqp], [pk(b)])
                    yield
                    for h in range(8):
                        b = 4 + h // 4
                        S.stt(SD[:, h, :], PS[b][:, (h % 4) * 128:(h % 4 + 1) * 128], wts[:, h:h + 1], MASK,
                              ALU.mult, ALU.mult, [pk(b), kW, "CM"], ["SD"])
                        if h % 4 == 3:
                            yield
                    for h in range(8):
                        b = 6 + h // 4
                        o = PS[b][:, (h % 4) * 128:(h % 4 + 1) * 128]
                        S.mm(o, vt[:, h * 128:(h + 1) * 128], SD[:, h, :], True, False, [kvt, "SD"], [pk(b)])
                        S.mm(o, CB[:, h // 2, :], qp[:, h, :], False, True, ["CB", kqp], [pk(b)])
                    yield
                    for h in range(8):
                        b = 4 + h // 4
                        o = PS[b][:, (h % 4) * 128:(h % 4 + 1) * 128]
                        S.mm(o, ONESB[:], SD[:, h, :], True, False, ["ONESB", "SD"], [pk(b)])
                        S.mm(o, NB[:, h // 2, :], qp[:, h, :], False, True, ["NB", kqp], [pk(b)])
                    yield
                    for g in range(2):
                        dn = flat(DEN[:, g * 4:(g + 1) * 4, :])
                        S.act(dn, PS[4 + g][:], AF.Abs, [pk(4 + g)], ["DEN%d" % g])
                        S.tt(dn, dn, flat(embs[:, g * 4:(g + 1) * 4, :]), ALU.max, ["DEN%d" % g, kE], ["DEN%d" % g])
                        yield
                        S.act(dn, dn, AF.Ln, ["DEN%d" % g], ["DEN%d" % g])
                        S.act(dn, dn, AF.Exp, ["DEN%d" % g], ["DEN%d" % g], scale=-1.0)
                        yield
                        S.tt(flat(HT[:, g * 4:(g + 1) * 4, :]), PS[6 + g][:], dn, ALU.mult,
                             [pk(6 + g), "DEN%d" % g], ["HT"])
                        yield
                    for c2 in range(4):
                        b = 6 + c2 // 2
                        S.mm(PS[b][:, (c2 % 2) * 256:(c2 % 2 + 1) * 256], kp[:, c2 * 128:(c2 + 1) * 128],
                             vt[:, c2 * 256:(c2 + 1) * 256], True, True, [kkp, kvt], [pk(b)])
                    for c2 in range(4):
                        S.mm(PS[4][:, c2:c2 + 1], kp[:, c2 * 128:(c2 + 1) * 128], ONESB[:, 0:1], True, True,
                             [kkp, "ONESB"], [pk(4)])
                    yield
                    for h in range(8):
                        r0 = (h % 2) * 64
                        c2 = h // 2
                        b = 6 + c2 // 2
                        col = (c2 % 2) * 256 + (h % 2) * 128
                        S.tt(CS[r0:r0 + 64, c2, 0:128], CS[r0:r0 + 64, c2, 0:128], PS[b][r0:r0 + 64, col:col + 128],
                             ALU.add, ["CS", pk(b)], ["CS"])
                        if h % 4 == 3:
                            yield
                    S.tt(CS[:, :, 128:129], CS[:, :, 128:129], PS[4][:, 0:4].unsqueeze(2), ALU.add,
                         ["CS", pk(4)], ["CS"])
                    yield
                    S.act(flat(SQ2[:]), flat(HT[:]), AF.Square, ["HT"], ["SQ2"])
                    for h in range(8):
                        b = 4 + h // 4
                        S.mm(PS[b][:, (h % 4) * 128:(h % 4 + 1) * 128], ONESB[:], SQ2[:, h, :], True, True,
                             ["ONESB", "SQ2"], [pk(b)])
                    yield
                    for g in range(2):
                        rstd_act(flat(DEN[:, g * 4:(g + 1) * 4, :]), PS[4 + g][:], 1.0 / 128.0, [pk(4 + g)],
                                 "DEN%d" % g)
                        yield
                    for h in range(8):
                        S.stt(HT[:, h, :], HT[:, h, :], V("ml_hg", h), DEN[:, h, :], ALU.mult, ALU.mult,
                              ["HT", "DEN%d" % (h // 4), "VEC"], ["HT"])
                        if h % 4 == 3:
                            yield
                    S.tt(flat(HB[:]), flat(HT[:]), flat(og[:]), ALU.mult, ["HT", kog], ["HB"])
                    yield
                    for d in range(8):
                        b = 6 + d // 4
                        for k in range(8):
                            S.mm(PS[b][:, (d % 4) * 128:(d % 4 + 1) * 128], WO[:, k, d * 128:(d + 1) * 128], HB[:, k, :],
                                 k == 0, k == 7, ["MWO", "HB"], [pk(b)])
                        if d % 2 == 1:
                            yield
                    for g in range(2):
                        S.act(flat(HM[:, g * 4:(g + 1) * 4, :]), PS[6 + g][:], AF.Identity, [pk(6 + g)], ["MHM"])
                        S.act(flat(SQB[:, g * 4:(g + 1) * 4, :]), PS[6 + g][:], AF.Square, [pk(6 + g)], ["MSQB"])
                        yield
                    postnorm("g_qm1", t0, T, HM, SQB, RSB, TMPN, 5, "MHM", xk="X%d" % i, sqk="MSQB", rk="MRSB")
                    yield

                pipeline(NT, stageA, stageB, ML_WB, ML_WA, ML_DA)
                S.barrier()

        S.barrier()
        for s in range(nseq):
            load_x(s)
            S.barrier()
            if "ab" in phases:
                mixer_ab(s)
            if "ffn0" in phases:
                ffn(0)
            if "ml" in phases:
                mixer_ml(s)
            if "ffn1" in phases:
                ffn(1)
            store_x(s)
        S.final_wait("sp", "xst")
    return nc


_W_NAMES = ("ab_w_in", "ab_w_out", "ml_w_in", "ml_w_out", "ffn_w_gate", "ffn_w_up", "ffn_w_down")


def kernel(**inputs):
    x = np.asarray(inputs["x"], np.float32)
    B = x.shape[0]
    consts = _host_consts(inputs)
    shared = dict(consts)
    shared["ab_w_in"] = np.ascontiguousarray(np.asarray(inputs["ab_w_in"], np.float32)[0])
    shared["ab_w_out"] = np.ascontiguousarray(np.asarray(inputs["ab_w_out"], np.float32)[0])
    shared["ml_w_in"] = np.ascontiguousarray(np.asarray(inputs["ml_w_in"], np.float32)[0])
    shared["ml_w_out"] = np.ascontiguousarray(np.asarray(inputs["ml_w_out"], np.float32)[0])
    for n in ("ffn_w_gate", "ffn_w_up", "ffn_w_down"):
        shared[n] = np.ascontiguousarray(np.asarray(inputs[n], np.float32))
    xT = np.ascontiguousarray(x.transpose(0, 2, 1))
    per = B // NCORES
    in_maps = []
    for c in range(NCORES):
        m = dict(shared)
        m["xT"] = np.ascontiguousarray(xT[c * per:(c + 1) * per])
        in_maps.append(m)
    nc = build_program(nseq=per, seq=x.shape[1])
    res = run_bass_kernel_spmd(nc, in_maps, core_ids=list(range(NCORES)))
    yT = np.concatenate([np.asarray(r["yT"]) for r in res.results], axis=0)
    return np.ascontiguousarray(yT.transpose(0, 2, 1)).astype(np.float32)
```

```python
import math
from contextlib import ExitStack
import numpy as np
import concourse.bass as bass
import concourse.mybir as mybir
from concourse.bass_utils import run_bass_kernel_spmd

F32 = mybir.dt.float32
BF16 = mybir.dt.bfloat16
AF = mybir.ActivationFunctionType
ALU = mybir.AluOpType

NCORES = 8
D = 1024
SEQ = 2048
NSEQ = 2
DFF = 2816
NF = DFF // 128
EPS = 1e-6
ML_IN = 3088
import os
ML_STOP = int(os.environ.get('ML_STOP', '0'))
NOIL = int(os.environ.get('NOIL', '0'))
AB_W1, AB_W2, AB_WB, AB_WA, AB_DA = [int(v) for v in os.environ.get('AB_IL', '1,1,1,1,0').split(',')]
ML_WB, ML_WA, ML_DA = [int(v) for v in os.environ.get('ML_IL', '1,1,0').split(',')]


class Sched:
    ENG = ("pe", "act", "dve", "pool", "sp")

    def __init__(self, nc, es):
        self.nc = nc
        self.es = es
        self.eng = {"pe": nc.tensor, "act": nc.scalar, "dve": nc.vector, "pool": nc.gpsimd, "sp": nc.sync}
        self.sem = {}
        self.mult = {}
        self.cnt = {}
        self.clock = {e: {} for e in self.ENG}
        self.snap = {}
        self.lastw = {}
        self.readers = {}
        for e in ("pe", "act", "dve", "pool"):
            self._chan(e, 1)

    def _chan(self, name, mult):
        if name not in self.sem:
            self.sem[name] = self.es.enter_context(self.nc.semaphore("s_" + name))
            self.mult[name] = mult
            self.cnt[name] = 0

    def _wait(self, e, ch, idx):
        ck = self.clock[e]
        if ck.get(ch, 0) >= idx:
            return
        self.eng[e].wait_ge(self.sem[ch], idx * self.mult[ch])
        ck[ch] = idx
        sn = self.snap.get((ch, idx))
        if sn:
            for c2, v2 in sn.items():
                if ck.get(c2, 0) < v2:
                    ck[c2] = v2

    def _deps(self, e, reads, writes):
        for k in reads:
            w = self.lastw.get(k)
            if w is not None:
                self._wait(e, w[0], w[1])
        for k in writes:
            w = self.lastw.get(k)
            if w is not None and w[0] != e:
                self._wait(e, w[0], w[1])
            rd = self.readers.get(k)
            if rd:
                for ch, idx in rd.items():
                    if ch != e:
                        self._wait(e, ch, idx)

    def _register(self, ch, idx, reads, writes):
        for k in reads:
            rd = self.readers.setdefault(k, {})
            if rd.get(ch, 0) < idx:
                rd[ch] = idx
        for k in writes:
            self.lastw[k] = (ch, idx)
            self.readers[k] = {}

    def op(self, e, meth, reads, writes, signal=True, **kw):
        self._deps(e, reads, writes)
        inst = getattr(self.eng[e], meth)(**kw)
        idx = self.cnt[e] + 1
        if signal:
            self.cnt[e] = idx
            inst.then_inc(self.sem[e], 1)
            sn = dict(self.clock[e])
            sn[e] = idx - 1
            self.snap[(e, idx)] = sn
        self._register(e, idx, reads, writes)
        return inst

    def dma(self, e, chan, out, in_, reads, writes):
        ch = "d_" + chan
        self._chan(ch, 16)
        self._deps(e, reads, writes)
        inst = self.eng[e].dma_start(out=out, in_=in_)
        self.cnt[ch] += 1
        idx = self.cnt[ch]
        inst.then_inc(self.sem[ch], 16)
        self.snap[(ch, idx)] = dict(self.clock[e])
        self._register(ch, idx, reads, writes)

    def barrier(self):
        for e in self.ENG:
            for ch, c in self.cnt.items():
                if c > 0 and ch != e:
                    self._wait(e, ch, c)
        self.lastw.clear()
        self.readers.clear()

    def final_wait(self, e, chan):
        ch = "d_" + chan
        self._wait(e, ch, self.cnt[ch])

    def mm(self, out, lhsT, rhs, start, stop, reads, writes, sig=None):
        return self.op("pe", "matmul", reads, writes, signal=(stop if sig is None else sig), out=out, lhsT=lhsT,
                       rhs=rhs, start=start, stop=stop)

    def act(self, out, in_, func, reads, writes, bias=None, scale=1.0):
        kw = dict(out=out, in_=in_, func=func, scale=scale)
        if bias is not None:
            kw["bias"] = bias
        return self.op("act", "activation", reads, writes, **kw)

    def tt(self, out, in0, in1, op, reads, writes, e="dve"):
        return self.op(e, "tensor_tensor", reads, writes, out=out, in0=in0, in1=in1, op=op)

    def stt(self, out, in0, scalar, in1, op0, op1, reads, writes, e="dve"):
        return self.op(e, "scalar_tensor_tensor", reads, writes, out=out, in0=in0, scalar=scalar,
                       in1=in1, op0=op0, op1=op1)

    def ts(self, out, in_, scalar, op, reads, writes, e="dve"):
        return self.op(e, "tensor_single_scalar", reads, writes, out=out, in_=in_, scalar=scalar, op=op)

    def cp(self, out, in_, reads, writes, e="dve"):
        return self.op(e, "tensor_copy", reads, writes, out=out, in_=in_)

    def scan(self, out, d0, d1, init, op0, op1, reads, writes):
        return self.op("dve", "tensor_tensor_scan", reads, writes, out=out, data0=d0, data1=d1,
                       initial=init, op0=op0, op1=op1)

    def recip(self, out, in_, reads, writes):
        return self.op("dve", "reciprocal", reads, writes, out=out, in_=in_)

    def memset(self, ap, val, writes, e="dve"):
        return self.op(e, "memset", [], writes, ap=ap, constant=val)


def _vec_layout():
    names = [("eps", 1), ("one", 1), ("ln8", 1)]
    for l in range(2):
        names += [("g_pm%d" % l, 8), ("g_qm%d" % l, 8), ("g_pf%d" % l, 8), ("g_qf%d" % l, 8)]
    names += [("ab_b", 16), ("ccw", 124), ("ccb", 4), ("lng", 4), ("lnb", 4), ("lcw", 16), ("lcb", 4),
              ("lba", 4), ("lbx", 4), ("lam", 4), ("ml_bq", 4), ("ml_bk", 4), ("ml_bo", 8), ("ml_hg", 8),
              ("ml_bi", 1), ("ml_bf", 1), ("pairsel", 4)]
    off = {}
    o = 0
    for n, c in names:
        off[n] = (o, c)
        o += c
    return off, o


VOFF, NV = _vec_layout()
NSEL = 8 * 128 + 8 * 128 + 128 + 8


def _fm(v, n):
    return np.ascontiguousarray(np.asarray(v, np.float32).reshape(n, 128).T)


def _host_consts(inp):
    vecs = np.zeros((128, NV), np.float32)

    def put(name, arr):
        o, c = VOFF[name]
        vecs[:arr.shape[0], o:o + c] = arr

    put("eps", np.full((128, 1), EPS, np.float32))
    put("one", np.ones((128, 1), np.float32))
    put("ln8", np.full((128, 1), math.log(0.125), np.float32))
    for l in range(2):
        put("g_pm%d" % l, _fm(inp["pre_mix_g"][l], 8))
        put("g_qm%d" % l, _fm(inp["post_mix_g"][l], 8))
        put("g_pf%d" % l, _fm(inp["pre_ffn_g"][l], 8))
        put("g_qf%d" % l, _fm(inp["post_ffn_g"][l], 8))
    put("ab_b", _fm(inp["ab_b_in"][0], 16))
    ccw = np.asarray(inp["conf_conv_w"][0], np.float32)
    put("ccw", np.ascontiguousarray(ccw.reshape(31, 4, 128).transpose(2, 1, 0).reshape(128, 124)))
    put("ccb", _fm(inp["conf_conv_b"][0], 4))
    put("lng", _fm(inp["conf_ln_g"][0], 4))
    put("lnb", _fm(inp["conf_ln_b"][0], 4))
    lcw = np.asarray(inp["lru_conv_w"][0], np.float32)
    put("lcw", np.ascontiguousarray(lcw.reshape(4, 4, 128).transpose(2, 1, 0).reshape(128, 16)))
    put("lcb", _fm(inp["lru_conv_b"][0], 4))
    put("lba", _fm(np.asarray(inp["lru_b_a"][0]).reshape(-1), 4))
    put("lbx", _fm(np.asarray(inp["lru_b_x"][0]).reshape(-1), 4))
    put("lam", _fm(inp["lru_lambda"][0], 4))
    mb = np.asarray(inp["ml_b_in"][0], np.float32)
    put("ml_bq", _fm(mb[0:512], 4))
    put("ml_bk", _fm(mb[512:1024], 4))
    put("ml_bo", _fm(mb[2048:3072], 8))
    put("ml_hg", _fm(inp["ml_head_g"][0], 8))
    put("ml_bi", mb[3072:3080].reshape(8, 1))
    put("ml_bf", mb[3080:3088].reshape(8, 1))
    ps = np.zeros((8, 4), np.float32)
    for h in range(8):
        ps[h, h // 2] = 1.0
    put("pairsel", ps)

    cm = np.zeros((128, 2, 128), np.float32)
    cm[:, 0, :] = np.eye(128, dtype=np.float32)
    cm[:, 1, :] = np.triu(np.ones((128, 128), np.float32))

    sel = np.zeros((8, NSEL), np.float32)
    for h in range(8):
        c2, hh = h // 2, h % 2
        sel[h, h * 128 + hh * 64: h * 128 + hh * 64 + 64] = 1.0
        sel[h, 1024 + h * 128: 1024 + (h + 1) * 128] = 1.0
        sel[h, 2048 + hh * 64: 2048 + hh * 64 + 64] = 1.0
        sel[h, 2176 + h] = 1.0
    bbc = np.ascontiguousarray(np.broadcast_to(mb[512:2048][None, :], (128, 1536))).astype(np.float32)

    lruw = np.zeros((128, 2, 4, 128), np.float32)
    for gi, nm in enumerate(("lru_w_a", "lru_w_x")):
        w = np.asarray(inp[nm][0], np.float32)
        for h in range(8):
            c, hh = h // 2, h % 2
            lruw[hh * 64:(hh + 1) * 64, gi, c, hh * 64:(hh + 1) * 64] = w[h]
    mgw = np.ascontiguousarray(np.asarray(inp["ml_w_in"][0], np.float32)[:, 3072:3088])
    return dict(vecs=vecs, cm=cm, sel=sel, bbc=bbc, lruw=lruw, mgw=mgw)


def build_program(nseq=NSEQ, seq=SEQ, phases=("ab", "ffn0", "ml", "ffn1")):
    nc = bass.Bass("TRN2", target_bir_lowering=False)
    dr = {}

    def din(name, shape):
        dr[name] = nc.dram_tensor(name, list(shape), F32, kind="ExternalInput").ap()
        return dr[name]

    xT = din("xT", [nseq, D, seq])
    vecs_d = din("vecs", [128, NV])
    cm_d = din("cm", [128, 2, 128])
    sel_d = din("sel", [8, NSEL])
    bbc_d = din("bbc", [128, 1536])
    lruw_d = din("lruw", [128, 2, 4, 128])
    mgw_d = din("mgw", [D, 16])
    abwi = din("ab_w_in", [D, 2048])
    abwo = din("ab_w_out", [D, D])
    mlwi = din("ml_w_in", [D, ML_IN])
    mlwo = din("ml_w_out", [D, D])
    fwg = din("ffn_w_gate", [2, D, DFF])
    fwu = din("ffn_w_up", [2, D, DFF])
    fwd = din("ffn_w_down", [2, DFF, D])
    yT = nc.dram_tensor("yT", [nseq, D, seq], F32, kind="ExternalOutput").ap()

    with ExitStack() as es:
        S = Sched(nc, es)

        uid = [0]

        def sb(name, shape, dt, st=None):
            uid[0] += 1
            return (st or es).enter_context(nc.sbuf_tensor("%s_%d" % (name, uid[0]), list(shape), dt))

        PS = [es.enter_context(nc.psum_tensor("ps%d" % i, [128, 512], F32)) for i in range(8)]

        def pk(i):
            return "ps%d" % i

        X = sb("X", [128, 8, seq], F32)
        VEC = sb("VEC", [128, NV], F32)
        CM = sb("CM", [128, 2, 128], F32)
        IDB = sb("IDB", [128, 128], BF16)
        ONESB = sb("ONESB", [128, 128], BF16)
        AVG512 = sb("AVG512", [128, 128], BF16)
        SELB = sb("SELB", [8, NSEL], BF16)
        ONESROW = sb("ONESROW", [8, 128], F32)
        CL = sb("CL", [128, 8], F32)
        NBF = sb("NBF", [8, 1], F32)

        def V(name, c=None, rows=128):
            o, n = VOFF[name]
            if c is None:
                return VEC[0:rows, o:o + n]
            return VEC[0:rows, o + c:o + c + 1]

        S.dma("sp", "c0", VEC[:], vecs_d, [], ["VEC"])
        S.dma("sp", "c0", CM[:], cm_d, [], ["CM"])
        S.dma("pool", "c1", SELB[:], sel_d, [], ["SELB"])
        S.cp(IDB[:], CM[:, 0, :], ["CM"], ["IDB"])
        S.memset(ONESB[:], 1.0, ["ONESB"])
        S.memset(AVG512[:], 1.0 / 512.0, ["AVG512"])
        S.memset(ONESROW[:], 1.0, ["ONESROW"])
        tmpc = sb("tmpc", [128, 4], F32)
        S.act(tmpc[:], V("lam"), AF.Exp, ["VEC"], ["tmpc"], scale=-1.0)
        S.act(tmpc[:], tmpc[:], AF.Ln, ["tmpc"], ["tmpc"], bias=V("one", 0))
        S.ts(CL[:, 0:4], tmpc[:], -8.0, ALU.mult, ["tmpc"], ["CL"])
        S.ts(CL[:, 4:8], tmpc[:], -16.0, ALU.mult, ["tmpc"], ["CL"])
        S.ts(NBF[:], V("ml_bf", 0, 8), -1.0, ALU.mult, ["VEC"], ["NBF"])
        MASK = CM[:, 1, :]

        def load_x(s):
            for c in range(8):
                S.dma("sp", "xld", X[:, c, :], xT[s, c * 128:(c + 1) * 128, :], [], ["X"])

        def store_x(s):
            for c in range(8):
                S.dma("sp", "xst", yT[s, c * 128:(c + 1) * 128, :], X[:, c, :], ["X"], [])

        def il(*gens):
            gens = list(gens)
            while gens:
                for g in list(gens):
                    try:
                        next(g)
                        yield
                    except StopIteration:
                        gens.remove(g)

        def ilw(specs):
            specs = [[g, w, d] for g, w, d in specs]
            rnd = 0
            while specs:
                for sp in list(specs):
                    if rnd < sp[2]:
                        continue
                    for _ in range(sp[1]):
                        try:
                            next(sp[0])
                            yield
                        except StopIteration:
                            specs.remove(sp)
                            break
                rnd += 1

        def run(g):
            for _ in g:
                pass

        def pipeline(n, stageA, stageB, wB=1, wA=1, dA=0):
            run(stageA(0))
            for i in range(n):
                if i + 1 < n and not NOIL:
                    run(ilw([(stageB(i), wB, 0), (stageA(i + 1), wA, dA)]))
                elif i + 1 < n:
                    run(stageB(i))
                    run(stageA(i + 1))
                else:
                    run(stageB(i))

        def rstd_act(out, in_, scale, reads, wkey):
            S.act(out, in_, AF.Ln, list(reads) + ["VEC"], [wkey], bias=V("eps", 0), scale=scale)
            S.act(out, out, AF.Exp, [wkey], [wkey], scale=-0.5)

        def prenorm(gname, t0, T, XN, SQ, RSTD, xn_key, bank, extra_w=(), xk="X", sqk="SQ", rk="RSTD"):
            S.act(SQ[:, :, 0:T], X[:, :, t0:t0 + T], AF.Square, [xk], [sqk] + list(extra_w))
            for c in range(8):
                S.mm(PS[bank][:, 0:T], ONESB[:], SQ[:, c, 0:T], c == 0, c == 7, [sqk, "ONESB"], [pk(bank)])
            rstd_act(RSTD[:, 0:T], PS[bank][:, 0:T], 1.0 / D, [pk(bank)], rk)
            for c in range(8):
                S.stt(XN[:, c, 0:T], X[:, c, t0:t0 + T], V(gname, c), RSTD[:, 0:T], ALU.mult, ALU.mult,
                      [xk, rk, "VEC"], [xn_key] + list(extra_w))

        def postnorm(gname, t0, T, HM, SQ, RSTD, TMP, bank, hm_key, stats_done=False, xk="X", sqk="SQ",
                     rk="RSTD", tk="TMPN"):
            if not stats_done:
                for c in range(8):
                    S.mm(PS[bank][:, 0:T], ONESB[:], SQ[:, c, 0:T], c == 0, c == 7, [sqk, "ONESB"], [pk(bank)])
            rstd_act(RSTD[:, 0:T], PS[bank][:, 0:T], 1.0 / D, [pk(bank)], rk)
            for c in range(8):
                ti = c % len(TMP)
                S.stt(TMP[ti][:, 0:T], HM[:, c, 0:T], V(gname, c), RSTD[:, 0:T], ALU.mult, ALU.mult,
                      [hm_key, rk, "VEC"], [tk + str(ti)])
                S.tt(X[:, c, t0:t0 + T], X[:, c, t0:t0 + T], TMP[ti][:, 0:T], ALU.add, [xk, tk + str(ti)], [xk])

        def mixer_ab(s):
            T = 256
            NT = seq // T
            with ExitStack() as st:
                WI = sb("WI", [128, 8, 2048], BF16, st)
                WO = sb("WO", [128, 8, 1024], BF16, st)
                LW = sb("LW", [128, 2, 4, 128], BF16, st)
                XN = sb("XN", [128, 8, T], BF16, st)
                SQA = sb("SQA", [128, 8, T], BF16, st)
                SQB = sb("SQB", [128, 8, T], BF16, st)
                RSA = sb("RSA", [128, T], F32, st)
                RSB = sb("RSB", [128, T], F32, st)
                TMPN = [sb("TMPN%d" % i, [128, T], F32, st) for i in range(2)]
                DG = [sb("DG%d" % i, [128, 31, 128], BF16, st) for i in range(2)]
                DGL = sb("DGL", [128, 16, 128], BF16, st)
                CI = [sb("CI%d" % i, [128, 4, 30 + T], BF16, st) for i in range(2)]
                RI = [sb("RI%d" % i, [128, 4, 3 + T], BF16, st) for i in range(2)]
                GG = [sb("GG%d" % i, [128, 4, T], BF16, st) for i in range(2)]
                SG = sb("SG", [128, T], F32, st)
                UY = sb("UY", [128, 8 * T], F32, st)
                HM = UY[:].rearrange("p (c t) -> p c t", c=8)
                YC = UY[:, 0:4 * T].rearrange("p (c t) -> p c t", c=4)
                YC2 = UY[:, 4 * T:6 * T].bitcast(BF16).rearrange("p (c t) -> p c t", c=4)
                YCB = UY[:, 6 * T:8 * T].bitcast(BF16).rearrange("p (c t) -> p c t", c=4)
                LN1 = sb("LN1", [128, T], F32, st)
                LN2 = sb("LN2", [128, T], F32, st)
                ZT = [sb("ZT%d" % i, [128, T], F32, st) for i in range(2)]
                YCAT = sb("YCAT", [128, 8, T], BF16, st)
                XR = sb("XR", [128, 4, T], F32, st)
                XRB = sb("XRB", [128, 4, T], BF16, st)
                LR = [sb("LR%d" % i, [128, T], F32, st) for i in range(4)]
                LI = [sb("LI%d" % i, [128, T], F32, st) for i in range(4)]
                LS = [sb("LS%d" % i, [128, T], F32, st) for i in range(4)]
                HH = [sb("HH%d" % i, [128, T], F32, st) for i in range(2)]
                HST = sb("HST", [128, 4], F32, st)
                print("ab sbuf remaining", nc.sbuf_bytes_remaining)

                for k in range(8):
                    S.dma("pool", "wmi", WI[:, k, :], abwi[k * 128:(k + 1) * 128, :], [], ["WI"])
                for k in range(8):
                    S.dma("pool", "wmo", WO[:, k, :], abwo[k * 128:(k + 1) * 128, :], [], ["WO"])
                S.dma("pool", "lw", LW[:], lruw_d, [], ["LW"])
                for j in range(16):
                    S.ts(DGL[:, j, :], IDB[:], V("lcw", j), ALU.mult, ["IDB", "VEC"], ["DGL"], e="pool")
                S.memset(HST[:], 0.0, ["HST"])
                o_ccw = VOFF["ccw"][0]

                def stageA(i):
                    par = i % 2
                    t0 = i * T
                    ci, ri, gg = CI[par], RI[par], GG[par]
                    kci, kri, kgg = "CI%d" % par, "RI%d" % par, "GG%d" % par
                    if i == 0:
                        S.memset(ci[:, :, 0:30], 0.0, [kci])
                        S.memset(ri[:, :, 0:3], 0.0, [kri])
                    else:
                        S.cp(ci[:, :, 0:30], CI[1 - par][:, :, T:T + 30], ["CI%d" % (1 - par)], [kci], e="pool")
                        S.cp(ri[:, :, 0:3], RI[1 - par][:, :, T:T + 3], ["RI%d" % (1 - par)], [kri], e="pool")
                    yield
                    prenorm("g_pm0", t0, T, XN, SQA, RSA, "XN", 2, xk="X%d" % i, sqk="SQA", rk="RSA")
                    yield
                    bankc = [0]

                    def proj(e):
                        b = bankc[0] % 3
                        bankc[0] += 1
                        for k in range(8):
                            S.mm(PS[b][:, 0:T], WI[:, k, e * 128:(e + 1) * 128], XN[:, k, :], k == 0, k == 7,
                                 ["WI", "XN"], [pk(b)])
                        return b

                    for c in range(4):
                        bg = proj(4 + c)
                        S.act(SG[:], PS[bg][:, 0:T], AF.Sigmoid, [pk(bg), "VEC"], ["SG"], bias=V("ab_b", 4 + c))
                        bv = proj(c)
                        S.stt(ci[:, c, 30:30 + T], PS[bv][:, 0:T], V("ab_b", c), SG[:], ALU.add, ALU.mult,
                              [pk(bv), "SG", "VEC"], [kci])
                        yield
                    for c in range(4):
                        b = proj(8 + c)
                        S.act(gg[:, c, :], PS[b][:, 0:T], AF.Gelu, [pk(b), "VEC"], [kgg], bias=V("ab_b", 8 + c))
                        yield
                    for c in range(4):
                        b = proj(12 + c)
                        S.act(ri[:, c, 3:3 + T], PS[b][:, 0:T], AF.Identity, [pk(b), "VEC"], [kri],
                              bias=V("ab_b", 12 + c))
                        yield

                def stageB1(i):
                    par = i % 2
                    ci, kci = CI[par], "CI%d" % par
                    for c in range(4):
                        wv = VEC[:, o_ccw + c * 31:o_ccw + (c + 1) * 31]
                        dg, kdg = DG[c % 2], "DG%d" % (c % 2)
                        S.tt(dg[:], IDB[:].unsqueeze(1).broadcast_to([128, 31, 128]),
                             wv.unsqueeze(2).broadcast_to([128, 31, 128]), ALU.mult, ["IDB", "VEC"], [kdg],
                             e=("pool" if c % 2 == 0 else "dve"))
                        if c == 0:
                            yield
                        for k in range(31):
                            S.mm(PS[3][:, 0:T], dg[:, k, :], ci[:, c, k:k + T], k == 0, k == 30,
                                 [kdg, kci], [pk(3)])
                        S.act(YC[:, c, :], PS[3][:, 0:T], AF.Identity, [pk(3), "VEC"], ["YC", "HM"], bias=V("ccb", c))
                        S.act(YC2[:, c, :], PS[3][:, 0:T], AF.Square, [pk(3), "VEC"], ["YC2", "HM"], bias=V("ccb", c))
                        S.cp(YCB[:, c, :], YC[:, c, :], ["YC"], ["YCB", "HM"], e="pool")
                        yield
                    for c in range(4):
                        S.mm(PS[4][:, 0:T], AVG512[:], YCB[:, c, :], c == 0, c == 3, ["YCB", "AVG512"], [pk(4)])
                    for c in range(4):
                        S.mm(PS[5][:, 0:T], AVG512[:], YC2[:, c, :], c == 0, c == 3, ["YC2", "AVG512"], [pk(5)])
                    yield
                    S.act(LN1[:], PS[4][:, 0:T], AF.Square, [pk(4)], ["LN1"])
                    S.tt(LN1[:], PS[5][:, 0:T], LN1[:], ALU.subtract, [pk(5), "LN1"], ["LN1"])
                    S.ts(LN1[:], LN1[:], 0.0, ALU.max, ["LN1"], ["LN1"])
                    yield
                    rstd_act(LN2[:], LN1[:], 1.0, ["LN1"], "LN2")
                    yield
                    for c in range(4):
                        zt, kz = ZT[c % 2], "ZT%d" % (c % 2)
                        S.tt(zt[:], YC[:, c, :], PS[4][:, 0:T], ALU.subtract, ["YC", pk(4)], [kz])
                        S.tt(zt[:], zt[:], LN2[:], ALU.mult, [kz, "LN2"], [kz])
                        S.act(YCAT[:, c, :], zt[:], AF.Silu, [kz, "VEC"], ["YCATa"], bias=V("lnb", c),
                              scale=V("lng", c))
                        yield

                def stageB2(i):
                    par = i % 2
                    ri, gg = RI[par], GG[par]
                    kri, kgg = "RI%d" % par, "GG%d" % par
                    for c in range(4):
                        for k in range(4):
                            S.mm(PS[6][:, 0:T], DGL[:, c * 4 + k, :], ri[:, c, k:k + T], k == 0, k == 3,
                                 ["DGL", kri], [pk(6)])
                        S.act(XR[:, c, :], PS[6][:, 0:T], AF.Identity, [pk(6), "VEC"], ["XR%d" % c], bias=V("lcb", c))
                        S.cp(XRB[:, c, :], XR[:, c, :], ["XR%d" % c], ["XRB%d" % c], e="pool")
                        yield
                    for hb in range(2):
                        cs = (2 * hb, 2 * hb + 1)
                        for j, c in enumerate(cs):
                            S.mm(PS[6][:, j * T:(j + 1) * T], LW[:, 0, c, :], XRB[:, c, :], True, True,
                                 ["LW", "XRB%d" % c], [pk(6)])
                            S.mm(PS[7][:, j * T:(j + 1) * T], LW[:, 1, c, :], XRB[:, c, :], True, True,
                                 ["LW", "XRB%d" % c], [pk(7)])
                        yield
                        for j, c in enumerate(cs):
                            S.act(LR[c][:], PS[6][:, j * T:(j + 1) * T], AF.Sigmoid, [pk(6), "VEC"], ["LR%d" % c],
                                  bias=V("lba", c))
                            S.act(LI[c][:], PS[7][:, j * T:(j + 1) * T], AF.Sigmoid, [pk(7), "VEC"], ["LI%d" % c],
                                  bias=V("lbx", c))
                        yield
                        for c in cs:
                            S.act(LS[c][:], LR[c][:], AF.Exp, ["LR%d" % c, "CL"], ["LS%d" % c], scale=CL[:, 4 + c:5 + c])
                            S.act(LR[c][:], LR[c][:], AF.Exp, ["LR%d" % c, "CL"], ["LR%d" % c], scale=CL[:, c:c + 1])
                            S.tt(LI[c][:], LI[c][:], XR[:, c, :], ALU.mult, ["LI%d" % c, "XR%d" % c], ["LI%d" % c])
                        yield
                        for c in cs:
                            S.op("dve", "tensor_scalar", ["LS%d" % c], ["LS%d" % c], out=LS[c][:], in0=LS[c][:],
                                 scalar1=-1.0, scalar2=1.0, op0=ALU.mult, op1=ALU.add)
                            S.ts(LS[c][:], LS[c][:], 1e-30, ALU.max, ["LS%d" % c], ["LS%d" % c])
                        yield
                        for c in cs:
                            S.act(LS[c][:], LS[c][:], AF.Ln, ["LS%d" % c], ["LS%d" % c])
                        for c in cs:
                            S.act(LS[c][:], LS[c][:], AF.Exp, ["LS%d" % c], ["LS%d" % c], scale=0.5)
                        yield
                        for j, c in enumerate(cs):
                            S.tt(LI[c][:], LI[c][:], LS[c][:], ALU.mult, ["LI%d" % c, "LS%d" % c], ["LI%d" % c])
                            S.scan(HH[j][:], LR[c][:], LI[c][:], HST[:, c:c + 1], ALU.mult, ALU.add,
                                   ["LR%d" % c, "LI%d" % c, "HST"], ["HH%d" % j])
                        yield
                        for j, c in enumerate(cs):
                            S.cp(HST[:, c:c + 1], HH[j][:, T - 1:T], ["HH%d" % j], ["HST"])
                            S.tt(YCAT[:, 4 + c, :], HH[j][:], gg[:, c, :], ALU.mult, ["HH%d" % j, kgg], ["YCATb"])
                        yield

                def stageB3(i):
                    t0 = i * T
                    for d in range(8):
                        b = 3 + d % 2
                        for k in range(8):
                            S.mm(PS[b][:, 0:T], WO[:, k, d * 128:(d + 1) * 128], YCAT[:, k, :], k == 0, k == 7,
                                 ["WO", "YCATa", "YCATb"], [pk(b)])
                        S.act(HM[:, d, :], PS[b][:, 0:T], AF.Identity, [pk(b)], ["HM", "YC", "YC2", "YCB"])
                        S.act(SQB[:, d, :], PS[b][:, 0:T], AF.Square, [pk(b)], ["SQB"])
                        yield
                    postnorm("g_qm0", t0, T, HM, SQB, RSB, TMPN, 5, "HM", xk="X%d" % i, sqk="SQB", rk="RSB")
                    yield

                def stageB(i):
                    yield from ilw([(stageB1(i), AB_W1, 0), (stageB2(i), AB_W2, 0)])
                    yield from stageB3(i)

                pipeline(NT, stageA, stageB, AB_WB, AB_WA, AB_DA)
                S.barrier()

        def ffn(l):
            TP = 1024
            NH = TP // 512
            NP = seq // TP
            with ExitStack() as st:
                HM = sb("FHM", [128, 8, TP], F32, st)
                XN = sb("FXN", [128, 8, TP], BF16, st)
                RSTD = sb("FRSTD", [128, 512], F32, st)
                TMPN = [sb("FTMPN", [128, 512], F32, st)]
                ACTB = sb("ACTB", [128, NF, TP], BF16, st)
                WGU = [sb("WGU%d" % i, [128, 2, 8, 256], BF16, st) for i in range(2)]
                WD = [sb("WD%d" % i, [128, NF, 128], BF16, st) for i in range(3)]
                SGF = [sb("SGF%d" % i, [128, 512], F32, st) for i in range(2)]
                SQT = [sb("SQT%d" % i, [128, 512], BF16, st) for i in range(2)]
                print("ffn sbuf remaining", nc.sbuf_bytes_remaining)
                wg_v = fwg[l].rearrange("(k p) f -> p k f", p=128)
                wu_v = fwu[l].rearrange("(k p) f -> p k f", p=128)
                wd_v = fwd[l].rearrange("(f p) d -> p f d", p=128)
                sqc = [0]

                def ld_gu(j):
                    i = j % 2
                    S.dma("pool", "wgu%d" % i, WGU[i][:, 0, :, :], wg_v[:, :, j * 256:(j + 1) * 256], [], ["WGU%d" % i])
                    S.dma("pool", "wgu%d" % i, WGU[i][:, 1, :, :], wu_v[:, :, j * 256:(j + 1) * 256], [], ["WGU%d" % i])

                def ld_d(d):
                    i = d % 3
                    S.dma("pool", "wd%d" % i, WD[i][:], wd_v[:, :, d * 128:(d + 1) * 128], [], ["WD%d" % i])

                def fprenorm(p, banks):
                    for h2 in range(NH):
                        t0 = p * TP + h2 * 512
                        bank = banks[h2]
                        for c in range(8):
                            sqt = SQT[sqc[0] % 2]
                            qk = "SQT%d" % (sqc[0] % 2)
                            sqc[0] += 1
                            S.act(sqt[:], X[:, c, t0:t0 + 512], AF.Square, ["X"], [qk])
                            S.mm(PS[bank][:], ONESB[:], sqt[:], c == 0, c == 7, [qk, "ONESB"], [pk(bank)], sig=True)
                        rstd_act(RSTD[:], PS[bank][:], 1.0 / D, [pk(bank)], "RSTD")
                        for c in range(8):
                            S.stt(XN[:, c, h2 * 512:(h2 + 1) * 512], X[:, c, t0:t0 + 512], V("g_pf%d" % l, c), RSTD[:],
                                  ALU.mult, ALU.mult, ["X", "RSTD", "VEC"], ["FXN"])

                ld_gu(0)
                ld_gu(1)
                fprenorm(0, (6, 7))
                for p in range(NP):
                    p0 = p * TP
                    cnt = 0
                    for j in range(NF // 2):
                        i = j % 2
                        for fc in range(2):
                            f = 2 * j + fc
                            for h2 in range(NH):
                                bg = (cnt % 2) * 2
                                bu = bg + 1
                                sgf = SGF[cnt % 2]
                                sk = "SGF%d" % (cnt % 2)
                                cnt += 1
                                for k in range(8):
                                    S.mm(PS[bg][:], WGU[i][:, 0, k, fc * 128:(fc + 1) * 128],
                                         XN[:, k, h2 * 512:(h2 + 1) * 512], k == 0, k == 7,
                                         ["WGU%d" % i, "FXN"], [pk(bg)])
                                for k in range(8):
                                    S.mm(PS[bu][:], WGU[i][:, 1, k, fc * 128:(fc + 1) * 128],
                                         XN[:, k, h2 * 512:(h2 + 1) * 512], k == 0, k == 7,
                                         ["WGU%d" % i, "FXN"], [pk(bu)])
                                S.act(sgf[:], PS[bg][:], AF.Silu, [pk(bg)], [sk])
                                S.tt(ACTB[:, f, h2 * 512:(h2 + 1) * 512], sgf[:], PS[bu][:], ALU.mult,
                                     [sk, pk(bu)], ["ACTB"])
                        if j + 2 < NF // 2:
                            ld_gu(j + 2)
                        if j == 0:
                            ld_d(0)
                            ld_d(1)
                            ld_d(2)
                    if p + 1 < NP:
                        fprenorm(p + 1, (0, 1))
                    pend = []
                    for d in range(8):
                        i = d % 3
                        for h2 in range(NH):
                            b = 4 + h2
                            for f in range(NF):
                                S.mm(PS[b][:], WD[i][:, f, :], ACTB[:, f, h2 * 512:(h2 + 1) * 512], f == 0, f == NF - 1,
                                     ["WD%d" % i, "ACTB"], [pk(b)])
                            for (psq, pqk, ph2, pd) in pend:
                                S.mm(PS[6 + ph2][:], ONESB[:], psq[:], pd == 0, pd == 7, [pqk, "ONESB"],
                                     [pk(6 + ph2)], sig=True)
                            pend = []
                            sqt = SQT[sqc[0] % 2]
                            qk = "SQT%d" % (sqc[0] % 2)
                            sqc[0] += 1
                            S.act(sqt[:], PS[b][:], AF.Square, [pk(b)], [qk])
                            S.act(HM[:, d, h2 * 512:(h2 + 1) * 512], PS[b][:], AF.Identity, [pk(b)], ["HM"])
                            pend.append((sqt, qk, h2, d))
                        if d + 3 < 8:
                            ld_d(d + 3)
                        if d == 1 and p + 1 < NP:
                            ld_gu(0)
                            ld_gu(1)
                    for (psq, pqk, ph2, pd) in pend:
                        S.mm(PS[6 + ph2][:], ONESB[:], psq[:], pd == 0, pd == 7, [pqk, "ONESB"], [pk(6 + ph2)], sig=True)
                    for h2 in range(NH):
                        postnorm("g_qf%d" % l, p0 + h2 * 512, 512, HM[:, :, h2 * 512:(h2 + 1) * 512], None, RSTD,
                                 TMPN, 6 + h2, "HM", stats_done=True)
                S.barrier()

        def mixer_ml(s):
            T = 128
            NT = seq // T
            with ExitStack() as st:
                WI = sb("MWI", [128, 8, 3072], BF16, st)
                WO = sb("MWO", [128, 8, 1024], BF16, st)
                WGm = sb("MWG", [128, 8, 16], BF16, st)
                BBC = sb("BBC", [128, 1536], F32, st)
                XN = sb("MXN", [128, 8, T], BF16, st)
                SQA = sb("MSQA", [128, 8, T], BF16, st)
                SQB = sb("MSQB", [128, 8, T], BF16, st)
                RSA = sb("MRSA", [128, T], F32, st)
                RSB = sb("MRSB", [128, T], F32, st)
                TMPN = [sb("MTMPN%d" % i, [128, T], F32, st) for i in range(2)]
                FI = sb("FI", [8, T], F32, st)
                SPt = sb("SPt", [8, T], F32, st)
                Gr = sb("Gr", [8, T], F32, st)
                Ar = sb("Ar", [8, T], F32, st)
                Pr = sb("Pr", [8, T], F32, st)
                Mr = sb("Mr", [8, T], F32, st)
                E8 = sb("E8", [8, T], BF16, st)
                Wr = sb("Wr", [8, T], BF16, st)
                EMIN = sb("EMIN", [8, T], BF16, st)
                GST = sb("GST", [8, 1], F32, st)
                PST = sb("PST", [8, 1], F32, st)
                NPC = sb("NPC", [8, 1], F32, st)
                PCL = sb("PCL", [8, 1], F32, st)
                DEC = sb("DEC", [8, 1], F32, st)
                DR = sb("DR", [8, 4], BF16, st)
                EBs = sb("EBs", [128, 8, T], F32, st)
                KTt = sb("KTt", [128, 512], F32, st)
                EMBs = [sb("EMBs%d" % i, [128, 8, T], F32, st) for i in range(2)]
                WTs = [sb("WTs%d" % i, [128, 8], F32, st) for i in range(2)]
                DBs = [sb("DBs%d" % i, [128, 4], F32, st) for i in range(2)]
                QP = [sb("QP%d" % i, [128, 8, T], BF16, st) for i in range(2)]
                KF = [sb("KF%d" % i, [128, 4, T], BF16, st) for i in range(2)]
                KP = [sb("KP%d" % i, [128, 512], BF16, st) for i in range(2)]
                VT = [sb("VT%d" % i, [128, 1024], BF16, st) for i in range(2)]
                OG = [sb("OG%d" % i, [128, 8, T], BF16, st) for i in range(2)]
                CS = sb("CS", [128, 4, 129], F32, st)
                CB = sb("CB", [128, 4, 128], BF16, st)
                NB = sb("NB", [128, 4, 128], BF16, st)
                SD = sb("SD", [128, 8, T], BF16, st)
                DEN = sb("DEN", [128, 8, T], F32, st)
                HT = sb("HT", [128, 8, T], F32, st)
                SQ2 = sb("SQ2", [128, 8, T], BF16, st)
                HB = sb("HB", [128, 8, T], BF16, st)
                HM = sb("MHM", [128, 8, T], F32, st)
                print("ml sbuf remaining", nc.sbuf_bytes_remaining)

                for k in range(8):
                    S.dma("pool", "wmi", WI[:, k, :], mlwi[k * 128:(k + 1) * 128, 0:3072], [], ["MWI"])
                for k in range(8):
                    S.dma("pool", "wmo", WO[:, k, :], mlwo[k * 128:(k + 1) * 128, :], [], ["MWO"])
                S.dma("pool", "lw", WGm[:], mgw_d.rearrange("(k p) e -> p k e", p=128), [], ["MWG"])
                S.dma("sp", "c0", BBC[:], bbc_d, [], ["BBC"])
                S.memset(CS[:], 0.0, ["CS"])
                S.memset(GST[:], 0.0, ["GST"])
                S.memset(PST[:], 0.0, ["PST"])
                SELPH = SELB[:, 0:1024].rearrange("p (c q) -> p c q", c=8)
                SELH = SELB[:, 1024:2048].rearrange("p (c q) -> p c q", c=8)
                SELODD = SELB[:, 2048:2176]
                I8 = SELB[:, 2176:2184]

                def flat(ap):
                    return ap.rearrange("p c t -> p (c t)")

                def gates(i):
                    par = i % 2
                    kE, kW, kD = "EMBs%d" % par, "WTs%d" % par, "DBs%d" % par
                    for k in range(8):
                        S.mm(PS[0][0:8, 0:T], WGm[:, k, 0:8], XN[:, k, :], k == 0, k == 7, ["MWG", "MXN"], [pk(0)])
                    for k in range(8):
                        S.mm(PS[0][0:8, 128:128 + T], WGm[:, k, 8:16], XN[:, k, :], k == 0, k == 7,
                             ["MWG", "MXN"], [pk(0)])
                    yield
                    S.act(FI[:], PS[0][0:8, 0:T], AF.Identity, [pk(0), "VEC"], ["FI"], bias=V("ml_bi", 0, 8))
                    S.act(SPt[:], PS[0][0:8, 128:128 + T], AF.Exp, [pk(0), "NBF"], ["SPt"], bias=NBF[:], scale=-1.0)
                    S.act(SPt[:], SPt[:], AF.Ln, ["SPt", "VEC"], ["SPt"], bias=V("one", 0, 8))
                    yield
                    S.scan(Gr[:], ONESROW[:, 0:T], SPt[:], GST[:], ALU.mult, ALU.subtract,
                           ["ONESROW", "SPt", "GST"], ["Gr"])
                    yield
                    S.cp(GST[:], Gr[:, T - 1:T], ["Gr"], ["GST"])
                    S.tt(Ar[:], FI[:], Gr[:], ALU.subtract, ["FI", "Gr"], ["Ar"])
                    yield
                    S.scan(Pr[:], Ar[:], Ar[:], PST[:], ALU.max, ALU.max, ["Ar", "PST"], ["Pr"])
                    yield
                    S.ts(NPC[:], Pr[:, T - 1:T], -1.0, ALU.mult, ["Pr"], ["NPC"])
                    S.ts(PCL[:], Pr[:, T - 1:T], math.log(0.125), ALU.add, ["Pr"], ["PCL"])
                    S.tt(Mr[:], Gr[:], Pr[:], ALU.add, ["Gr", "Pr"], ["Mr"])
                    yield
                    S.act(DEC[:], PST[:], AF.Exp, ["PST", "NPC"], ["DEC"], bias=NPC[:])
                    S.act(E8[:], Pr[:], AF.Exp, ["Pr", "PCL"], ["E8"], bias=PCL[:], scale=-1.0)
                    S.act(Wr[:], Ar[:], AF.Exp, ["Ar", "NPC"], ["Wr"], bias=NPC[:])
                    S.act(EMIN[:], Mr[:], AF.Exp, ["Mr"], ["EMIN"], scale=-1.0)
                    yield
                    S.cp(PST[:], Pr[:, T - 1:T], ["Pr"], ["PST"])
                    S.ts(DR[:], V("pairsel", None, 8), DEC[:], ALU.mult, ["VEC", "DEC"], ["DR"])
                    yield
                    S.mm(PS[0][:, 0:8], Wr[:], I8, True, True, ["Wr", "SELB"], [pk(0)])
                    S.mm(PS[0][:, 8:12], SELODD, DR[:], True, True, ["DR", "SELB"], [pk(0)])
                    yield
                    S.cp(WTs[par][:], PS[0][:, 0:8], [pk(0)], [kW])
                    S.cp(DBs[par][:], PS[0][:, 8:12], [pk(0)], [kD])
                    yield
                    for g in range(2):
                        for h in range(4 * g, 4 * g + 4):
                            S.mm(PS[1][:, (h % 4) * 128:(h % 4 + 1) * 128], SELPH[:, h, :], E8[:], True, True,
                                 ["SELB", "E8"], [pk(1)])
                        for h in range(4 * g, 4 * g + 4):
                            S.mm(PS[0][:, (h % 4) * 128:(h % 4 + 1) * 128], SELH[:, h, :], EMIN[:], True, True,
                                 ["SELB", "EMIN"], [pk(0)])
                        yield
                        S.act(flat(EBs[:, 4 * g:4 * g + 4, :]), PS[1][:], AF.Identity, [pk(1)], ["EBs"])
                        S.cp(flat(EMBs[par][:, 4 * g:4 * g + 4, :]), PS[0][:], [pk(0)], [kE])
                        yield

                def kvo(i):
                    par = i % 2
                    kf, vt, og = KF[par], VT[par], OG[par]
                    kkf, kvt, kog = "KF%d" % par, "VT%d" % par, "OG%d" % par
                    for hv in range(2):
                        for k in range(8):
                            S.mm(PS[2 + hv][:], XN[:, k, :], WI[:, k, 1024 + hv * 512:1024 + (hv + 1) * 512],
                                 k == 0, k == 7, ["MWI", "MXN"], [pk(2 + hv)])
                        S.tt(vt[:, hv * 512:(hv + 1) * 512], PS[2 + hv][:], BBC[:, 512 + hv * 512:512 + (hv + 1) * 512],
                             ALU.add, [pk(2 + hv), "BBC"], [kvt])
                        yield
                    for k in range(8):
                        S.mm(PS[2][:], XN[:, k, :], WI[:, k, 512:1024], k == 0, k == 7, ["MWI", "MXN"], [pk(2)])
                    S.tt(KTt[:], PS[2][:], BBC[:, 0:512], ALU.add, [pk(2), "BBC"], ["KTt"])
                    yield
                    for c2 in range(4):
                        for k in range(8):
                            S.mm(PS[3][:, c2 * 128:(c2 + 1) * 128], WI[:, k, 512 + c2 * 128:512 + (c2 + 1) * 128],
                                 XN[:, k, :], k == 0, k == 7, ["MWI", "MXN"], [pk(3)])
                        yield
                    for c2 in range(4):
                        S.act(kf[:, c2, :], PS[3][:, c2 * 128:(c2 + 1) * 128], AF.Identity, [pk(3), "VEC"], [kkf],
                              bias=V("ml_bk", c2))
                    yield
                    for hc in range(8):
                        b = 2 + hc // 4
                        for k in range(8):
                            S.mm(PS[b][:, (hc % 4) * 128:(hc % 4 + 1) * 128],
                                 WI[:, k, 2048 + hc * 128:2048 + (hc + 1) * 128], XN[:, k, :], k == 0, k == 7,
                                 ["MWI", "MXN"], [pk(b)])
                        yield
                    for hc in range(8):
                        b = 2 + hc // 4
                        S.act(og[:, hc, :], PS[b][:, (hc % 4) * 128:(hc % 4 + 1) * 128], AF.Sigmoid,
                              [pk(b), "VEC"], [kog], bias=V("ml_bo", hc))
                        if hc % 4 == 3:
                            yield

                def stageA(i):
                    par = i % 2
                    t0 = i * T
                    prenorm("g_pm1", t0, T, XN, SQA, RSA, "MXN", 3, xk="X%d" % i, sqk="MSQA", rk="MRSA")
                    yield
                    yield from il(gates(i), kvo(i))
                    S.tt(KP[par][:].rearrange("p (h d) -> p h d", h=8), KTt[:].rearrange("p (h d) -> p h d", h=8),
                         WTs[par][:].unsqueeze(2).broadcast_to([128, 8, 64]), ALU.mult, ["KTt", "WTs%d" % par],
                         ["KP%d" % par])
                    for c2 in range(4):
                        for k in range(8):
                            S.mm(PS[2][:, c2 * 128:(c2 + 1) * 128], WI[:, k, c2 * 128:(c2 + 1) * 128], XN[:, k, :],
                                 k == 0, k == 7, ["MWI", "MXN"], [pk(2)])
                        yield
                    for h in range(8):
                        c2 = h // 2
                        S.stt(QP[par][:, h, :], PS[2][:, c2 * 128:(c2 + 1) * 128], V("ml_bq", c2), EBs[:, h, :],
                              ALU.add, ALU.mult, [pk(2), "EBs", "VEC"], ["QP%d" % par])
                        if h % 4 == 3:
                            yield

                def stageB(i):
                    par = i % 2
                    t0 = i * T
                    qp, kf, kp, vt, og = QP[par], KF[par], KP[par], VT[par], OG[par]
                    kqp, kkf, kkp, kvt, kog = "QP%d" % par, "KF%d" % par, "KP%d" % par, "VT%d" % par, "OG%d" % par
                    embs, wts, dbs = EMBs[par], WTs[par], DBs[par]
                    kE, kW, kD = "EMBs%d" % par, "WTs%d" % par, "DBs%d" % par
                    for c2 in range(4):
                        S.ts(CS[:, c2, :], CS[:, c2, :], dbs[:, c2:c2 + 1], ALU.mult, ["CS", kD], ["CS"])
                    S.cp(CB[:], CS[:, :, 0:128], ["CS"], ["CB"])
                    S.cp(NB[:], CS[:, :, 128:129].broadcast_to([128, 4, 128]), ["CS"], ["NB"])
                    yield
                    for h in range(8):
                        b = 4 + h // 4
                        S.mm(PS[b][:, (h % 4) * 128:(h % 4 + 1) * 128], kf[:, h // 2, :], qp[:, h, :], True, True,
                             [kkf, kqp], [pk(b)])
                    yield
                    for h in range(8):
                        b = 4 + h // 4
                        S.stt(SD[:, h, :], PS[b][:, (h % 4) * 128:(h % 4 + 1) * 128], wts[:, h:h + 1], MASK,
                              ALU.mult, ALU.mult, [pk(b), kW, "CM"], ["SD"])
                        if h % 4 == 3:
                            yield
                    for h in range(8):
                        b = 6 + h // 4
                        o = PS[b][:, (h % 4) * 128:(h % 4 + 1) * 128]
                        S.mm(o, vt[:, h * 128:(h + 1) * 128], SD[:, h, :], True, False, [kvt, "SD"], [pk(b)])
                        S.mm(o, CB[:, h // 2, :], qp[:, h, :], False, True, ["CB", kqp], [pk(b)])
                    yield
                    for h in range(8):
                        b = 4 + h // 4
                        o = PS[b][:, (h % 4) * 128:(h % 4 + 1) * 128]
                        S.mm(o, ONESB[:], SD[:, h, :], True, False, ["ONESB", "SD"], [pk(b)])
                        S.mm(o, NB[:, h // 2, :], qp[:, h, :], False, True, ["NB", kqp], [pk(b)])
                    yield
                    for g in range(2):
                        dn = flat(DEN[:, g * 4:(g + 1) * 4, :])
                        S.act(dn, PS[4 + g][:], AF.Abs, [pk(4 + g)], ["DEN%d" % g])
                        S.tt(dn, dn, flat(embs[:, g * 4:(g + 1) * 4, :]), ALU.max, ["DEN%d" % g, kE], ["DEN%d" % g])
                        yield
                        S.act(dn, dn, AF.Ln, ["DEN%d" % g], ["DEN%d" % g])
                        S.act(dn, dn, AF.Exp, ["DEN%d" % g], ["DEN%d" % g], scale=-1.0)
                        yield
                        S.tt(flat(HT[:, g * 4:(g + 1) * 4, :]), PS[6 + g][:], dn, ALU.mult,
                             [pk(6 + g), "DEN%d" % g], ["HT"])
                        yield
                    for c2 in range(4):
                        b = 6 + c2 // 2
                        S.mm(PS[b][:, (c2 % 2) * 256:(c2 % 2 + 1) * 256], kp[:, c2 * 128:(c2 + 1) * 128],
                             vt[:, c2 * 256:(c2 + 1) * 256], True, True, [kkp, kvt], [pk(b)])
                    for c2 in range(4):
                        S.mm(PS[4][:, c2:c2 + 1], kp[:, c2 * 128:(c2 + 1) * 128], ONESB[:, 0:1], True, True,
                             [kkp, "ONESB"], [pk(4)])
                    yield
                    for h in range(8):
                        r0 = (h % 2) * 64
                        c2 = h // 2
                        b = 6 + c2 // 2
                        col = (c2 % 2) * 256 + (h % 2) * 128
                        S.tt(CS[r0:r0 + 64, c2, 0:128], CS[r0:r0 + 64, c2, 0:128], PS[b][r0:r0 + 64, col:col + 128],
                             ALU.add, ["CS", pk(b)], ["CS"])
                        if h % 4 == 3:
                            yield
                    S.tt(CS[:, :, 128:129], CS[:, :, 128:129], PS[4][:, 0:4].unsqueeze(2), ALU.add,
                         ["CS", pk(4)], ["CS"])
                    yield
                    S.act(flat(SQ2[:]), flat(HT[:]), AF.Square, ["HT"], ["SQ2"])
                    for h in range(8):
                        b = 4 + h // 4
                        S.mm(PS[b][:, (h % 4) * 128:(h % 4 + 1) * 128], ONESB[:], SQ2[:, h, :], True, True,
                             ["ONESB", "SQ2"], [pk(b)])
                    yield
                    for g in range(2):
                        rstd_act(flat(DEN[:, g * 4:(g + 1) * 4, :]), PS[4 + g][:], 1.0 / 128.0, [pk(4 + g)],
                                 "DEN%d" % g)
                        yield
                    for h in range(8):
                        S.stt(HT[:, h, :], HT[:, h, :], V("ml_hg", h), DEN[:, h, :], ALU.mult, ALU.mult,
                              ["HT", "DEN%d" % (h // 4), "VEC"], ["HT"])
                        if h % 4 == 3:
                            yield
                    S.tt(flat(HB[:]), flat(HT[:]), flat(og[:]), ALU.mult, ["HT", kog], ["HB"])
                    yield
                    for d in range(8):
                        b = 6 + d // 4
                        for k in range(8):
                            S.mm(PS[b][:, (d % 4) * 128:(d % 4 + 1) * 128], WO[:, k, d * 128:(d + 1) * 128], HB[:, k, :],
                                 k == 0, k == 7, ["MWO", "HB"], [pk(b)])
                        if d % 2 == 1:
                            yield
                    for g in range(2):
                        S.act(flat(HM[:, g * 4:(g + 1) * 4, :]), PS[6 + g][:], AF.Identity, [pk(6 + g)], ["MHM"])
                        S.act(flat(SQB[:, g * 4:(g + 1) * 4, :]), PS[6 + g][:], AF.Square, [pk(6 + g)], ["MSQB"])
                        yield
                    postnorm("g_qm1", t0, T, HM, SQB, RSB, TMPN, 5, "MHM", xk="X%d" % i, sqk="MSQB", rk="MRSB")
                    yield

                pipeline(NT, stageA, stageB, ML_WB, ML_WA, ML_DA)
                S.barrier()

        S.barrier()
        for s in range(nseq):
            load_x(s)
            S.barrier()
            if "ab" in phases:
                mixer_ab(s)
            if "ffn0" in phases:
                ffn(0)
            if "ml" in phases:
                mixer_ml(s)
            if "ffn1" in phases:
                ffn(1)
            store_x(s)
        S.final_wait("sp", "xst")
    return nc


_W_NAMES = ("ab_w_in", "ab_w_out", "ml_w_in", "ml_w_out", "ffn_w_gate", "ffn_w_up", "ffn_w_down")


def kernel(**inputs):
    x = np.asarray(inputs["x"], np.float32)
    B = x.shape[0]
    consts = _host_consts(inputs)
    shared = dict(consts)
    shared["ab_w_in"] = np.ascontiguousarray(np.asarray(inputs["ab_w_in"], np.float32)[0])
    shared["ab_w_out"] = np.ascontiguousarray(np.asarray(inputs["ab_w_out"], np.float32)[0])
    shared["ml_w_in"] = np.ascontiguousarray(np.asarray(inputs["ml_w_in"], np.float32)[0])
    shared["ml_w_out"] = np.ascontiguousarray(np.asarray(inputs["ml_w_out"], np.float32)[0])
    for n in ("ffn_w_gate", "ffn_w_up", "ffn_w_down"):
        shared[n] = np.ascontiguousarray(np.asarray(inputs[n], np.float32))
    xT = np.ascontiguousarray(x.transpose(0, 2, 1))
    per = B // NCORES
    in_maps = []
    for c in range(NCORES):
        m = dict(shared)
        m["xT"] = np.ascontiguousarray(xT[c * per:(c + 1) * per])
        in_maps.append(m)
    nc = build_program(nseq=per, seq=x.shape[1])
    res = run_bass_kernel_spmd(nc, in_maps, core_ids=list(range(NCORES)))
    yT = np.concatenate([np.asarray(r["yT"]) for r in res.results], axis=0)
    return np.ascontiguousarray(yT.transpose(0, 2, 1)).astype(np.float32)
```
